# Optimizing a Trainium2 kernel written in Bass

```python
import jax, jax.numpy as jnp
from jax import lax
import numpy as np

D_MODEL = 1024
BATCH = 16
SEQ = 2048
DEPTH = 1

CHUNK = 64
MIX_WIDTH = D_MODEL
MLSTM_WIDTH = MIX_WIDTH // 2
MLSTM_HEADS = 4
MLSTM_HEAD_DIM = MLSTM_WIDTH // MLSTM_HEADS
MLSTM_CONV = 4
SB_WIDTH = MIX_WIDTH - MLSTM_WIDTH
SB_HEADS = 8
SB_HEAD_DIM = SB_WIDTH // SB_HEADS
SB_BLOCK = 128
D_FF = 2816
FFN_CONV = 3
EPS = 1e-6
IN_COLS = 4 * MLSTM_WIDTH + 2 * MLSTM_HEADS + 3 * SB_WIDTH

kernel_name = 'hybrid_mlstm_stickbreaking_convffn'


def rms_norm(x, w):
    xf = x.astype(jnp.float32)
    y = xf * lax.rsqrt(jnp.mean(xf * xf, axis=-1, keepdims=True) + EPS)
    return (y * w.astype(jnp.float32)).astype(x.dtype)


def causal_depthwise_conv(x, w, b):
    k_width, ch = w.shape
    y = lax.conv_general_dilated(x, w[:, None, :].astype(x.dtype), window_strides=(1,),
                                 padding=[(k_width - 1, 0)],
                                 dimension_numbers=('NWC', 'WIO', 'NWC'),
                                 feature_group_count=ch)
    return y + b.astype(x.dtype)


def split_heads(t, n_heads):
    b, s, w = t.shape
    return t.reshape(b, s, n_heads, w // n_heads).transpose(0, 2, 1, 3)


def merge_heads(t):
    b, h, s, d = t.shape
    return t.transpose(0, 2, 1, 3).reshape(b, s, h * d)


def mlstm_chunkwise(q, k, v, log_i, log_f):
    b, h, s, d = q.shape
    nc = s // CHUNK
    q = q.astype(jnp.float32)
    k = k.astype(jnp.float32) * (d ** -0.5)
    v = v.astype(jnp.float32)
    log_i = log_i.astype(jnp.float32)
    log_f = log_f.astype(jnp.float32)

    def to_chunks(t):
        return jnp.moveaxis(t.reshape(b, h, nc, CHUNK, *t.shape[3:]), 2, 0)

    causal = jnp.tril(jnp.ones((CHUNK, CHUNK), dtype=bool))

    def step(carry, inp):
        c_state, n_state, m_state = carry
        qc, kc, vc, li, lf = inp
        bcum = jnp.cumsum(lf, axis=-1)
        d_log = jnp.where(causal, bcum[..., :, None] - bcum[..., None, :] + li[..., None, :], -jnp.inf)
        inter = bcum + m_state[..., None]
        m_t = jnp.maximum(jnp.max(d_log, axis=-1), inter)
        w_intra = jnp.exp(d_log - m_t[..., None])
        w_inter = jnp.exp(inter - m_t)
        scores = jnp.einsum('bhtd,bhsd->bhts', qc, kc) * w_intra
        num = (jnp.einsum('bhts,bhse->bhte', scores, vc)
               + w_inter[..., None] * jnp.einsum('bhtd,bhde->bhte', qc, c_state))
        den = jnp.sum(scores, axis=-1) + w_inter * jnp.einsum('bhtd,bhd->bht', qc, n_state)
        h_c = num / jnp.maximum(jnp.abs(den), jnp.exp(-m_t))[..., None]
        b_last = bcum[..., -1]
        g = b_last[..., None] - bcum + li
        a = b_last + m_state
        m_new = jnp.maximum(a, jnp.max(g, axis=-1))
        w_k = jnp.exp(g - m_new[..., None])
        decay = jnp.exp(a - m_new)
        c_new = decay[..., None, None] * c_state + jnp.einsum('bhs,bhsd,bhse->bhde', w_k, kc, vc)
        n_new = decay[..., None] * n_state + jnp.einsum('bhs,bhsd->bhd', w_k, kc)
        return (c_new, n_new, m_new), h_c

    init = (jnp.zeros((b, h, d, d), jnp.float32), jnp.zeros((b, h, d), jnp.float32),
            jnp.zeros((b, h), jnp.float32))
    _, hs = lax.scan(step, init, (to_chunks(q), to_chunks(k), to_chunks(v),
                                  to_chunks(log_i), to_chunks(log_f)))
    return jnp.moveaxis(hs, 0, 2).reshape(b, h, s, d)


def stick_breaking_attention(q, k, v):
    b, h, s, d = q.shape
    scale = d ** -0.5
    q = q.astype(jnp.float32)
    k = k.astype(jnp.float32)
    v = v.astype(jnp.float32)
    outs = []
    for blk in range(s // SB_BLOCK):
        t0 = blk * SB_BLOCK
        t1 = t0 + SB_BLOCK
        z = jnp.einsum('bhtd,bhsd->bhts', q[:, :, t0:t1], k[:, :, :t1]) * scale
        t_idx = t0 + jnp.arange(SB_BLOCK)[:, None]
        s_idx = jnp.arange(t1)[None, :]
        mask = s_idx < t_idx
        log_1mb = jnp.where(mask, jax.nn.log_sigmoid(-z), 0.0)
        rem = lax.cumsum(log_1mb, axis=3, reverse=True) - log_1mb
        attn = jnp.where(mask, jnp.exp(jax.nn.log_sigmoid(z) + rem), 0.0)
        outs.append(jnp.einsum('bhts,bhse->bhte', attn, v[:, :, :t1]))
    return jnp.concatenate(outs, axis=2)


def setup_inputs(seed: int = 0) -> dict:
    key = jax.random.key(seed)
    ks = jax.random.split(key, 20)
    f32 = jnp.float32

    def gain(k_, n):
        return 1.0 + 0.05 * jax.random.normal(k_, (DEPTH, n), f32)

    b_f = (jnp.linspace(3.0, 6.0, MLSTM_HEADS, dtype=f32)[None, :]
           + 0.1 * jax.random.normal(ks[6], (DEPTH, MLSTM_HEADS), f32))
    return {
        'x': jax.random.normal(ks[0], (BATCH, SEQ, D_MODEL), f32),
        'pre_mix_norm': gain(ks[1], D_MODEL),
        'w_in': jax.random.normal(ks[2], (DEPTH, D_MODEL, IN_COLS), f32) * D_MODEL ** -0.5,
        'mlstm_conv_w': jax.random.normal(ks[3], (DEPTH, MLSTM_CONV, 2 * MLSTM_WIDTH), f32) * MLSTM_CONV ** -0.5,
        'mlstm_conv_b': 0.01 * jax.random.normal(ks[4], (DEPTH, 2 * MLSTM_WIDTH), f32),
        'mlstm_b_i': 0.1 * jax.random.normal(ks[5], (DEPTH, MLSTM_HEADS), f32),
        'mlstm_b_f': b_f,
        'mlstm_norm': gain(ks[7], MLSTM_WIDTH),
        'w_out': jax.random.normal(ks[8], (DEPTH, MIX_WIDTH, D_MODEL), f32) * MIX_WIDTH ** -0.5,
        'post_mix_norm': gain(ks[9], D_MODEL),
        'pre_ffn_norm': gain(ks[10], D_MODEL),
        'w_up': jax.random.normal(ks[11], (DEPTH, D_MODEL, 2 * D_FF), f32) * D_MODEL ** -0.5,
        'ffn_conv_w': jax.random.normal(ks[12], (DEPTH, FFN_CONV, D_FF), f32) * FFN_CONV ** -0.5,
        'ffn_conv_b': 0.01 * jax.random.normal(ks[13], (DEPTH, D_FF), f32),
        'w_down': jax.random.normal(ks[14], (DEPTH, D_FF, D_MODEL), f32) * D_FF ** -0.5,
        'post_ffn_norm': gain(ks[15], D_MODEL),
    }


def reference(x, pre_mix_norm, w_in, mlstm_conv_w, mlstm_conv_b, mlstm_b_i, mlstm_b_f,
              mlstm_norm, w_out, post_mix_norm, pre_ffn_norm, w_up, ffn_conv_w, ffn_conv_b,
              w_down, post_ffn_norm):
    bsz, seq, _ = x.shape
    o1 = 2 * MLSTM_WIDTH
    o2 = o1 + MLSTM_WIDTH
    o3 = o2 + MLSTM_WIDTH
    o4 = o3 + MLSTM_HEADS
    o5 = o4 + MLSTM_HEADS
    o6 = o5 + SB_WIDTH
    o7 = o6 + SB_WIDTH
    for l in range(DEPTH):
        h = rms_norm(x, pre_mix_norm[l])
        proj = h @ w_in[l]
        qk_m = jax.nn.silu(causal_depthwise_conv(proj[..., :o1], mlstm_conv_w[l], mlstm_conv_b[l]))
        q_m = split_heads(qk_m[..., :MLSTM_WIDTH], MLSTM_HEADS)
        k_m = split_heads(qk_m[..., MLSTM_WIDTH:], MLSTM_HEADS)
        v_m = split_heads(proj[..., o1:o2], MLSTM_HEADS)
        o_gate = jax.nn.sigmoid(proj[..., o2:o3])
        log_i = (proj[..., o3:o4] + mlstm_b_i[l]).transpose(0, 2, 1)
        log_f = jax.nn.log_sigmoid(proj[..., o4:o5] + mlstm_b_f[l]).transpose(0, 2, 1)
        h_m = mlstm_chunkwise(q_m, k_m, v_m, log_i, log_f)
        h_m = rms_norm(h_m.transpose(0, 2, 1, 3),
                       mlstm_norm[l].reshape(MLSTM_HEADS, MLSTM_HEAD_DIM)).reshape(bsz, seq, MLSTM_WIDTH)
        h_m = (o_gate * h_m).astype(x.dtype)

        q_s = split_heads(proj[..., o5:o6], SB_HEADS)
        k_s = split_heads(proj[..., o6:o7], SB_HEADS)
        v_s = split_heads(proj[..., o7:], SB_HEADS)
        h_s = merge_heads(stick_breaking_attention(q_s, k_s, v_s)).astype(x.dtype)

        mix = jnp.concatenate([h_m, h_s], axis=-1) @ w_out[l]
        x = x + rms_norm(mix, post_mix_norm[l])

        h = rms_norm(x, pre_ffn_norm[l])
        gu = h @ w_up[l]
        gate = causal_depthwise_conv(gu[..., :D_FF], ffn_conv_w[l], ffn_conv_b[l])
        y = (jax.nn.gelu(gate, approximate=True) * gu[..., D_FF:]) @ w_down[l]
        x = x + rms_norm(y, post_ffn_norm[l])
    return x
```

```python
import contextlib
import math
import numpy as np
import concourse.bass as bass
import concourse.mybir as mybir
from concourse.bass_utils import run_bass_kernel_spmd

F32 = mybir.dt.float32
BF16 = mybir.dt.bfloat16
U8 = mybir.dt.uint8
AF = mybir.ActivationFunctionType
ALU = mybir.AluOpType

NCORES = 8
S = 2048
D = 1024
NT = S // 128
DFF = 2816
NFF = DFF // 128
INC = 3592
EPS = 1e-6
FB = 512
NDUMMY = 2


class Prog:
    ENGS = ("pe", "act", "dve", "pool", "sp")
    NDMA = 48

    def __init__(self, nc):
        self.nc = nc
        self.ops = {e: [] for e in self.ENGS}
        self.last_w = {}
        self.readers = {}
        self.ndma = 0
        self.alias_of = {}

    def alias(self, new_names, old_names):
        deps = set()
        old = set(old_names)
        for t, w in self.last_w.items():
            if t[0] in old and w is not None:
                deps.add(w)
        for t, rs in self.readers.items():
            if t[0] in old:
                deps.update(rs)
        deps = list(deps)
        new = set(new_names)
        for t in self.readers:
            if t[0] in new:
                self.readers[t] = self.readers[t] + deps
        for n in new_names:
            self.alias_of[n] = deps

    def _touch(self, t):
        if t not in self.last_w and t not in self.readers:
            self.last_w[t] = None
            self.readers[t] = list(self.alias_of.get(t[0], ()))

    def _dep(self, o, prod):
        if prod is None:
            return
        peng, pidx, pdma = prod
        if pdma is None and peng == o["eng"] and (peng == "pe" or pidx == o["idx"]):
            return
        o["waits"].add(prod)

    def op(self, eng, fn, reads=(), writes=(), dma=False):
        idx = len(self.ops[eng])
        o = {"eng": eng, "fn": fn, "waits": set(), "dma": None, "idx": idx, "inc": False}
        if dma:
            o["dma"] = self.ndma
            self.ndma += 1
        me = (eng, idx, o["dma"])
        writes = list(writes) + [t for t in reads if t[0] == "ps"]
        reads = [t for t in reads if t[0] != "ps"]
        for t in list(reads) + list(writes):
            self._touch(t)
        for t in reads:
            self._dep(o, self.last_w.get(t))
        for t in writes:
            self._dep(o, self.last_w.get(t))
            for r in self.readers.get(t, ()):
                if r != me:
                    self._dep(o, r)
        for t in reads:
            self.readers[t].append(me)
        for t in writes:
            self.last_w[t] = me
            self.readers[t] = []
        self.ops[eng].append(o)
        return me

    def emit(self, final_waits=()):
        nc = self.nc
        for e in self.ENGS:
            for o in self.ops[e]:
                for (pe, pi, pd) in o["waits"]:
                    if pd is None:
                        self.ops[pe][pi]["inc"] = True
        semval = {}
        for e in self.ENGS:
            c = 0
            for o in self.ops[e]:
                if o["dma"] is None and o["inc"]:
                    c += 1
                    semval[(e, o["idx"])] = c
        dma_slot, dma_val, dma_prev = {}, {}, {}
        slot_cnt = [0] * self.NDMA
        slot_last = [None] * self.NDMA
        order = [o for e in self.ENGS for o in self.ops[e] if o["dma"] is not None]
        order.sort(key=lambda o: o["dma"])
        half = self.NDMA // 2
        rr = {"hw": 0, "sw": 0}
        for o in order:
            if o["eng"] == "pool":
                s = half + rr["sw"] % half
                rr["sw"] += 1
            else:
                s = rr["hw"] % half
                rr["hw"] += 1
            slot_cnt[s] += 1
            dma_slot[o["dma"]] = s
            dma_val[o["dma"]] = 16 * slot_cnt[s]
            dma_prev[o["dma"]] = slot_last[s]
            slot_last[s] = o["dma"]

        with contextlib.ExitStack() as st:
            esem = {e: st.enter_context(nc.semaphore("s_" + e)) for e in ("pe", "act", "dve", "pool")}
            dsem = [st.enter_context(nc.semaphore("d%d" % i)) for i in range(self.NDMA)]
            block = st.enter_context(nc.Block())

            def run(ename, eng):
                seen = {}
                for o in self.ops[ename]:
                    waits = []
                    for (pe, pi, pd) in o["waits"]:
                        if pd is None:
                            waits.append((("e", pe), esem[pe], semval[(pe, pi)]))
                        else:
                            waits.append((("d", dma_slot[pd]), dsem[dma_slot[pd]], dma_val[pd]))
                    if o["dma"] is not None and dma_prev[o["dma"]] is not None:
                        pd = dma_prev[o["dma"]]
                        waits.append((("d", dma_slot[pd]), dsem[dma_slot[pd]], dma_val[pd]))
                    best = {}
                    for k, s, v in waits:
                        if seen.get(k, 0) >= v:
                            continue
                        if k not in best or best[k][1] < v:
                            best[k] = (s, v)
                    for k, (s, v) in best.items():
                        eng.wait_ge(s, v)
                        seen[k] = v
                    ins = o["fn"](eng)
                    if o["dma"] is not None:
                        ins.then_inc(dsem[dma_slot[o["dma"]]], 16)
                    elif o["inc"]:
                        ins.then_inc(esem[ename], 1)
                if ename == "sp":
                    for (pe, pi, pd) in final_waits:
                        eng.wait_ge(dsem[dma_slot[pd]], dma_val[pd])

            block.tensor(lambda eng: run("pe", eng))
            block.scalar(lambda eng: run("act", eng))
            block.vector(lambda eng: run("dve", eng))
            block.gpsimd(lambda eng: run("pool", eng))
            block.sync(lambda eng: run("sp", eng))


class Arena:
    def __init__(self, tensor, size):
        self.t = tensor
        self.size = size
        self.off = 0
        self.hi = 0

    def take(self, shape, dt, at=None):
        esz = {F32: 4, BF16: 2}[dt]
        n = int(np.prod(shape[1:])) * esz
        n_al = (n + 63) // 64 * 64
        if at is None:
            at = self.off
            self.off += n_al
        self.hi = max(self.hi, at + n_al)
        assert at + n_al <= self.size, ("SBUF arena overflow", at + n_al, self.size)
        v = self.t[:, at:at + n].bitcast(dt)
        if len(shape) == 3:
            v = v.rearrange("p (a b) -> p a b", a=shape[1])
        elif len(shape) == 4:
            v = v.rearrange("p (a b c) -> p a b c", a=shape[1], b=shape[2])
        return v, at


def build_program(nseq=2, do_ffn=True, debug_mix=False):
    nc = bass.Bass("TRN2", target_bir_lowering=False)
    dr = lambda name, shape, dt=F32, kind="ExternalInput": nc.dram_tensor(name, shape, dt, kind=kind).ap()
    x_d = dr("x", [nseq, S, D])
    w_in_d = dr("w_in", [D, INC])
    w_out_d = dr("w_out", [D, D])
    w_up_d = dr("w_up", [D, 2 * DFF])
    w_dn_d = dr("w_down", [DFF, D])
    g1T_d = dr("g1T", [128, 1024])
    g3T_d = dr("g3T", [128, 1024])
    g2b_d = dr("g2b", [128, 1024])
    g4b_d = dr("g4b", [128, 1024])
    gmb_d = dr("gmb", [128, 512])
    cwm_d = dr("cwm", [128, 32])
    cbm_d = dr("cbm", [128, 8])
    bif_d = dr("bif", [4, 2])
    cwf_d = dr("cwf", [128, NFF * 3])
    cbf_d = dr("cbf", [128, NFF])
    out_d = dr("out", [nseq, S, D], kind="ExternalOutput")
    dbg_d = dr("dbg", [nseq, 128, 8 * S], BF16, kind="ExternalOutput") if debug_mix else None

    P = Prog(nc)
    with contextlib.ExitStack() as st:
        ARENA = 206 * 1024
        arena_t = st.enter_context(nc.sbuf_tensor("arena", [128, ARENA], U8))
        A = Arena(arena_t, ARENA)
        psum_t = st.enter_context(nc.psum_tensor("psum", [128, 4096], F32))
        bank = [psum_t[:, i * 512:(i + 1) * 512] for i in range(8)]
        bankb = [b.bitcast(BF16) for b in bank]

        def PS(i):
            return ("ps", i)

        ident_b, _ = A.take([128, 128], BF16)
        ident_f, _ = A.take([128, 128], F32)
        maskM, _ = A.take([128, 128], BF16)
        maskS, _ = A.take([128, 128], F32)
        triU, _ = A.take([128, 128], BF16)
        comp, _ = A.take([128, 128], BF16)
        negm, _ = A.take([128, 128], BF16)
        sel, _ = A.take([128, 4, 128], F32)
        xt = [A.take([128, 1024], F32)[0] for _ in range(4)]
        xh = [A.take([128, 1024], BF16)[0] for _ in range(2)]
        stat, _ = A.take([128, 16], F32)
        persist_end = A.off

        def const_tri(ap, pattern, cm, op, base=0, val=1.0):
            P.op("pool", lambda e: e.memset(ap, val), writes=[("const",)])
            P.op("pool", lambda e: e.affine_select(out=ap, in_=ap, pattern=pattern, compare_op=op, fill=0.0,
                                                   base=base, channel_multiplier=cm),
                 reads=[("const",)], writes=[("const",)])

        const_tri(ident_b, [[-1, 128]], 1, ALU.is_equal)
        const_tri(ident_f, [[-1, 128]], 1, ALU.is_equal)
        const_tri(maskM, [[1, 128]], -1, ALU.is_ge)
        const_tri(maskS, [[1, 128]], -1, ALU.is_gt)
        const_tri(triU, [[-1, 128]], 1, ALU.is_ge)
        const_tri(comp, [[1, 128]], -1, ALU.is_gt)
        const_tri(sel[0:4], [[-1, 4], [0, 128]], 1, ALU.is_equal)
        const_tri(negm, [[-1, 128]], 1, ALU.is_ge, val=-2400.0)
        CONST = [("const",)]

        def rms_rstd(src_ap, src_tok, n, col, junk_ap, junk_tok):
            P.op("act", lambda e: e.activation(out=junk_ap, in_=src_ap, func=AF.Square,
                                               accum_out=stat[:, col:col + 1]),
                 reads=src_tok, writes=[junk_tok, ("stat", col)])
            P.op("act", lambda e: e.activation(out=stat[:, col + 1:col + 2], in_=stat[:, col:col + 1], func=AF.Ln,
                                               scale=1.0 / n, bias=EPS),
                 reads=[("stat", col)], writes=[("stat", col + 1)])
            P.op("act", lambda e: e.activation(out=stat[:, col + 2:col + 3], in_=stat[:, col + 1:col + 2],
                                               func=AF.Exp, scale=-0.5),
                 reads=[("stat", col + 1)], writes=[("stat", col + 2)])
            return stat[:, col + 2:col + 3], ("stat", col + 2)

        def norm_transpose(src_dram_tile, src_tok, b, gT, gT_tok, dstT, dst_tok, tcol):
            P.op("sp", lambda e: e.dma_start(out=xt[b], in_=src_dram_tile), reads=src_tok, writes=[("xt", b)], dma=True)
            rstd, rtok = rms_rstd(xt[b], [("xt", b)], D, 4 * b, xh[b], ("xh", b))
            P.op("dve", lambda e: e.tensor_scalar(out=xh[b], in0=xt[b], scalar1=rstd, scalar2=None, op0=ALU.mult),
                 reads=[("xt", b), rtok], writes=[("xh", b)])
            for c in range(8):
                P.op("pe", (lambda c: lambda e: e.transpose(out=bankb[7][:, c * 128:(c + 1) * 128],
                                                            in_=xh[b][:, c * 128:(c + 1) * 128], identity=ident_b))(c),
                     reads=[("xh", b)] + CONST, writes=[PS(7)])
            P.op("dve", lambda e: e.tensor_tensor(out=dstT[:, :, tcol:tcol + 128],
                                                  in0=bankb[7].rearrange("p (c t) -> p c t", c=8),
                                                  in1=gT.rearrange("p (c t) -> p c t", c=8), op=ALU.mult),
                 reads=[PS(7), gT_tok], writes=[dst_tok])

        def post_norm_residual(ybanks, b, gb, gb_tok, resid_tok_reads, out_dram_tile, out_tok, xb=None, junk=None):
            xb = b if xb is None else xb
            yap = psum_t[:, ybanks * 512:ybanks * 512 + 1024]
            ytok = [PS(ybanks), PS(ybanks + 1)]
            jap, jtok = junk if junk is not None else (xh[b], ("xh", b))
            rstd, rtok = rms_rstd(yap, ytok, D, 8 + 4 * b, jap, jtok)
            P.op("dve", lambda e: e.scalar_tensor_tensor(out=yap, in0=yap, scalar=rstd, in1=gb, op0=ALU.mult, op1=ALU.mult),
                 reads=ytok + [rtok, gb_tok], writes=[])
            P.op("dve", lambda e: e.tensor_tensor(out=xt[xb], in0=yap, in1=xt[xb], op=ALU.add),
                 reads=ytok + [("xt", xb)] + list(resid_tok_reads), writes=[("xt", xb)])
            return P.op("sp", lambda e: e.dma_start(out=out_dram_tile, in_=xt[xb]), reads=[("xt", xb)],
                        writes=out_tok, dma=True)

        persist_end = A.off

        A.off = persist_end
        region1_at = A.off
        hT, _ = A.take([128, 8, S], BF16)
        qk, _ = A.take([128, 8, S], BF16)
        raw0, raw_at = A.take([128, 3 + S], BF16)
        raw1, _ = A.take([128, 3 + S], BF16)
        diagM, _ = A.take([128, 8, 4, 128], BF16)
        rawdiag_end = A.off
        wB, wB_at = A.take([128, 8, 1032], BF16)
        region1_end = A.off
        mixT, mixT_at = A.take([128, 8, S], BF16)
        wA, _ = A.take([128, 8, 1024], BF16)
        region2_end = A.off
        g1T, _ = A.take([128, 1024], F32)
        g2b, _ = A.take([128, 1024], F32)
        gmb, _ = A.take([128, 512], F32)
        cwm, _ = A.take([128, 32], F32)
        cbm, _ = A.take([128, 8], F32)
        bif, _ = A.take([128, 2], F32)
        ifr, _ = A.take([128, NT, 8], F32)
        tokS, _ = A.take([128, NT, 12], F32)
        Cf, _ = A.take([128, 4, 129], F32)
        Cb, _ = A.take([128, 4, 129], BF16)
        K2 = [A.take([128, 4, 128], BF16)[0] for _ in range(2)]
        spT = [A.take([128, 512], BF16)[0] for _ in range(2)]
        hmt, _ = A.take([128, 512], BF16)
        vmt = [A.take([128, 4, 129], BF16)[0] for _ in range(2)]
        ogt = [A.take([128, 512], F32)[0] for _ in range(2)]
        decb, _ = A.take([128, 4, 16], F32)
        sm, _ = A.take([128, 64], F32)
        rsm, _ = A.take([128, 64], F32)
        mixer_end = A.off
        rows = [A.take([128, 1024], F32, at=raw_at + i * 4096)[0] for i in range(4)]
        assert raw_at + 4 * 4096 <= rawdiag_end
        vst, _ = A.take([128, NT, 512], BF16, at=mixT_at + 4 * S * 2)
        vs, _ = A.take([128, NT, 512], BF16, at=raw_at)
        assert raw_at + NT * 512 * 2 <= rawdiag_end
        Ebuf = [A.take([128, 512], F32, at=wB_at + i * 2048)[0] for i in range(3)]
        ERbuf = [A.take([128, 512], F32, at=wB_at + 6144 + i * 2048)[0] for i in range(2)]
        SPbuf = [A.take([128, 512], BF16, at=wB_at + 10240 + i * 1024)[0] for i in range(3)]
        ATbuf = [A.take([128, 512], BF16, at=wB_at + 13312 + i * 1024)[0] for i in range(3)]

        RAWGRP = ["raw0", "raw1", "diagM"]
        ATTGRP = ["E", "ER", "SP", "AT"]

        for (ap, d_ap, nm) in [(g1T, g1T_d, "g1T"), (g2b, g2b_d, "g2b"), (gmb, gmb_d, "gmb"), (cwm, cwm_d, "cwm"),
                               (cbm, cbm_d, "cbm")]:
            P.op("sp", (lambda ap, d_ap: lambda e: e.dma_start(out=ap, in_=d_ap))(ap, d_ap), writes=[(nm,)], dma=True)
        P.op("sp", lambda e: e.dma_start(out=bif[0:4, :], in_=bif_d), writes=[("bif",)], dma=True)

        def load_w(dst, dst_tok, src, c0, ncols, eng="pool"):
            for kc in range(8):
                P.op(eng, (lambda kc: lambda e: e.dma_start(out=dst[:, kc, 0:ncols],
                                                            in_=src[kc * 128:(kc + 1) * 128, c0:c0 + ncols]))(kc),
                     writes=[(dst_tok, kc)], dma=True)

        fin = []
        for sq in range(nseq):
            P.alias(["wB"], ATTGRP)
            load_w(wA, "wA", w_in_d, 0, 1024)
            load_w(wB, "wB", w_in_d, 1024, 1032)
            if sq == 0:
                for tt in range(NT):
                    norm_transpose(x_d[sq, tt * 128:(tt + 1) * 128, :], [], tt % 2, g1T, ("g1T",), hT, ("hT", tt), tt * 128)

            P.alias(RAWGRP, ["vs", "rows"])
            P.alias(["vst"], ["mixT"])
            for cc in range(8):
                for j in range(4):
                    P.op("dve", (lambda cc, j: lambda e: e.tensor_scalar(out=diagM[:, cc, j, :], in0=ident_b,
                                                                         scalar1=cwm[:, cc * 4 + j:cc * 4 + j + 1],
                                                                         scalar2=None, op0=ALU.mult))(cc, j),
                         reads=CONST + [("cwm",)], writes=[("diagM",)])
            for rb, rawb in enumerate((raw0, raw1)):
                P.op("pool", (lambda rawb: lambda e: e.memset(rawb[:, 0:3], 0.0))(rawb), writes=[("raw%d" % rb, "halo")])
            for cc in range(8):
                rb = cc % 2
                rawb = (raw0, raw1)[rb]
                for tb in range(4):
                    bk = tb % 2
                    for kc in range(8):
                        P.op("pe", (lambda kc, cc, tb, bk: lambda e: e.matmul(
                            bank[bk], lhsT=wA[:, kc, cc * 128:(cc + 1) * 128], rhs=hT[:, kc, tb * 512:(tb + 1) * 512],
                            start=(kc == 0), stop=(kc == 7)))(kc, cc, tb, bk),
                            reads=[("wA", kc)] + [("hT", tb * 4 + i) for i in range(4)], writes=[PS(bk)])
                    P.op("act", (lambda rawb, tb, bk: lambda e: e.copy(out=rawb[:, 3 + tb * 512:3 + (tb + 1) * 512],
                                                                        in_=bank[bk]))(rawb, tb, bk),
                         reads=[PS(bk)], writes=[("raw%d" % rb, tb)])
                for tb in range(4):
                    bk = 2 + tb % 2
                    for j in range(4):
                        rd = [("raw%d" % rb, tb)] + ([("raw%d" % rb, tb - 1)] if tb > 0 else [("raw%d" % rb, "halo")])
                        if tb < 3:
                            rd.append(("raw%d" % rb, tb))
                        P.op("pe", (lambda cc, j, tb, bk, rawb: lambda e: e.matmul(
                            bank[bk], lhsT=diagM[:, cc, j, :], rhs=rawb[:, tb * 512 + j:tb * 512 + j + 512],
                            start=(j == 0), stop=(j == 3)))(cc, j, tb, bk, rawb),
                            reads=rd + [("diagM",)], writes=[PS(bk)])
                    P.op("act", (lambda cc, tb, bk: lambda e: e.activation(
                        out=qk[:, cc, tb * 512:(tb + 1) * 512], in_=bank[bk], func=AF.Silu,
                        bias=cbm[:, cc:cc + 1]))(cc, tb, bk),
                        reads=[PS(bk), ("cbm",)], writes=[("qk", cc, tb)])
            for tt in range(NT):
                for kc in range(8):
                    P.op("pe", (lambda kc, tt: lambda e: e.matmul(
                        bank[4][:, 0:8], lhsT=hT[:, kc, tt * 128:(tt + 1) * 128], rhs=wB[:, kc, 1024:1032],
                        start=(kc == 0), stop=(kc == 7)))(kc, tt),
                        reads=[("wB", kc), ("hT", tt)], writes=[PS(4)])
                P.op("dve", (lambda tt: lambda e: e.tensor_copy(out=ifr[:, tt, :], in_=bank[4][:, 0:8]))(tt),
                     reads=[PS(4)], writes=[("ifr", tt)])

            for tt in range(NT):
                bk = 2 + tt % 2
                for kc in range(8):
                    P.op("pe", (lambda kc, tt, bk: lambda e: e.matmul(
                        bank[bk], lhsT=hT[:, kc, tt * 128:(tt + 1) * 128], rhs=wB[:, kc, 0:512], start=(kc == 0),
                        stop=(kc == 7)))(kc, tt, bk),
                        reads=[("wB", kc), ("hT", tt)], writes=[PS(bk)])
                if tt % 2:
                    P.op("act", (lambda tt, bk: lambda e: e.copy(out=vst[:, tt, :], in_=bank[bk]))(tt, bk),
                         reads=[PS(bk)], writes=[("vst", tt)])
                else:
                    P.op("dve", (lambda tt, bk: lambda e: e.tensor_copy(out=vst[:, tt, :], in_=bank[bk]))(tt, bk),
                         reads=[PS(bk)], writes=[("vst", tt)])

            P.alias(["rows"], RAWGRP)
            lnk = -0.5 * math.log(128.0)
            R = lambda i: ("rows", i)
            P.op("dve", lambda e: e.memset(rsm[0:4, 0:8], 0.0), writes=[("rsm",)])
            for hh in range(2):
                for tl in range(8):
                    tt = hh * 8 + tl
                    for g in range(2):
                        P.op("pe", (lambda tt, tl, g: lambda e: e.transpose(
                            out=psum_t[0:4, g * 1024 + tl * 128:g * 1024 + (tl + 1) * 128],
                            in_=ifr[:, tt, g * 4:(g + 1) * 4], identity=ident_f))(tt, tl, g),
                            reads=[("ifr", tt)] + CONST, writes=[PS(2 * g + tl // 4)])
                P.op("dve", lambda e: e.tensor_scalar(out=rows[0][0:4], in0=psum_t[0:4, 0:1024], scalar1=bif[0:4, 0:1],
                                                      scalar2=None, op0=ALU.add),
                     reads=[PS(0), PS(1), ("bif",)], writes=[R(0)])
                P.op("dve", lambda e: e.tensor_scalar(out=rows[1][0:4], in0=psum_t[0:4, 1024:2048],
                                                      scalar1=bif[0:4, 1:2], scalar2=None, op0=ALU.add),
                     reads=[PS(2), PS(3), ("bif",)], writes=[R(1)])
                P.op("act", lambda e: e.activation(out=rows[1][0:4], in_=rows[1][0:4], func=AF.Exp, scale=-1.0),
                     reads=[R(1)], writes=[R(1)])
                P.op("act", lambda e: e.activation(out=rows[1][0:4], in_=rows[1][0:4], func=AF.Ln, bias=1.0),
                     reads=[R(1)], writes=[R(1)])
                P.op("dve", lambda e: e.tensor_scalar(out=rows[1][0:4], in0=rows[1][0:4], scalar1=-0.5, scalar2=None,
                                                      op0=ALU.mult),
                     reads=[R(1)], writes=[R(1)])
                P.op("dve", lambda e: e.tensor_tensor_scan(out=rows[2][0:4], data0=rows[1][0:4], data1=rows[1][0:4],
                                                           initial=rsm[0:4, 0:1], op0=ALU.add, op1=ALU.add),
                     reads=[R(1), ("rsm",)], writes=[R(2)])
                P.op("dve", lambda e: e.tensor_tensor(out=rows[0][0:4], in0=rows[0][0:4], in1=rows[2][0:4],
                                                      op=ALU.subtract),
                     reads=[R(0), R(2)], writes=[R(0)])
                P.op("dve", lambda e: e.tensor_tensor_scan(out=rows[1][0:4], data0=rows[0][0:4], data1=rows[0][0:4],
                                                           initial=rsm[0:4, 1:2], op0=ALU.max, op1=ALU.max),
                     reads=[R(0), R(1), ("rsm",)], writes=[R(1)])
                P.op("dve", lambda e: e.tensor_copy(out=rsm[0:4, 8:9], in_=rsm[0:4, 1:2]), reads=[("rsm",)],
                     writes=[("rsm",)])
                P.op("dve", lambda e: e.tensor_copy(out=rsm[0:4, 16:24], in_=rows[1][0:4, 127:1024:128]),
                     reads=[R(1), ("rsm",)], writes=[("rsm",)])
                P.op("dve", lambda e: e.tensor_copy(out=rsm[0:4, 9:16], in_=rsm[0:4, 16:23]), reads=[("rsm",)],
                     writes=[("rsm",)])
                P.op("dve", lambda e: e.tensor_copy(out=rsm[0:4, 0:1], in_=rows[2][0:4, 1023:1024]),
                     reads=[R(2), ("rsm",)], writes=[("rsm",)])
                P.op("dve", lambda e: e.tensor_copy(out=rsm[0:4, 1:2], in_=rows[1][0:4, 1023:1024]),
                     reads=[R(1), ("rsm",)], writes=[("rsm",)])
                P.op("dve", (lambda hh: lambda e: e.tensor_tensor(out=rsm[0:4, 24 + 8 * hh:32 + 8 * hh],
                                                                  in0=rsm[0:4, 8:16], in1=rsm[0:4, 16:24],
                                                                  op=ALU.subtract))(hh),
                     reads=[("rsm",)], writes=[("rsm",)])
                for q, (src, rcol, op, sc, bi) in enumerate([(0, 8, ALU.subtract, 1.0, lnk), (0, 16, ALU.subtract, 1.0, lnk),
                                                             (2, 8, ALU.add, -1.0, 0.0)]):
                    P.op("dve", (lambda src, rcol, op: lambda e: e.tensor_tensor(
                        out=rows[3][0:4].rearrange("p (c t) -> p c t", c=8),
                        in0=rows[src][0:4].rearrange("p (c t) -> p c t", c=8),
                        in1=rsm[0:4, rcol:rcol + 8].unsqueeze(2).broadcast_to([4, 8, 128]), op=op))(src, rcol, op),
                        reads=[R(src), ("rsm",)], writes=[R(3)])
                    P.op("act", (lambda sc, bi: lambda e: e.activation(out=rows[3][0:4], in_=rows[3][0:4], func=AF.Exp,
                                                                        scale=sc, bias=bi))(sc, bi),
                         reads=[R(3)], writes=[R(3)])
                    for tl in range(8):
                        P.op("pe", (lambda tl, q: lambda e: e.transpose(
                            out=bank[4][:, tl * 12 + q * 4:tl * 12 + q * 4 + 4], in_=rows[3][0:4, tl * 128:(tl + 1) * 128],
                            identity=ident_f[0:4, 0:4]))(tl, q),
                            reads=[R(3)] + CONST, writes=[PS(4)])
                P.op("dve", (lambda hh: lambda e: e.tensor_copy(
                    out=tokS[:, hh * 8:(hh + 1) * 8, :], in_=bank[4][:, 0:96].rearrange("p (a b) -> p a b", a=8)))(hh),
                    reads=[PS(4)], writes=[("tokS", hh)])
            P.op("act", lambda e: e.activation(out=rsm[0:4, 24:40], in_=rsm[0:4, 24:40], func=AF.Exp),
                 reads=[("rsm",)], writes=[("rsm",)])
            for h in range(4):
                P.op("pe", (lambda h: lambda e: e.matmul(bank[5][:, h * 16:(h + 1) * 16], lhsT=sel[0:4, h, :],
                                                         rhs=rsm[0:4, 24:40], start=True, stop=True))(h),
                     reads=[("rsm",)] + CONST, writes=[PS(5)])
            P.op("dve", lambda e: e.tensor_copy(out=decb, in_=bank[5][:, 0:64].rearrange("p (a b) -> p a b", a=4)),
                 reads=[PS(5)], writes=[("decb",)])
            P.op("dve", lambda e: e.memset(Cf, 0.0), writes=[("Cf",)])
            P.op("pool", lambda e: e.memset(Cb, 0.0), writes=[("Cb",)])
            for b2 in range(2):
                P.op("pool", (lambda b2: lambda e: e.memset(vmt[b2][:, :, 128:129], 1.0))(b2), writes=[("vmt", "ones", b2)])

            def W1a(c):
                tb = c // 4
                cs = slice(c * 128, (c + 1) * 128)
                b2 = c % 2
                for (bk, c0) in ((7, 512),):
                    for kc in range(8):
                        P.op("pe", (lambda kc, bk, c0: lambda e: e.matmul(
                            bank[bk], lhsT=hT[:, kc, cs], rhs=wB[:, kc, c0:c0 + 512], start=(kc == 0),
                            stop=(kc == 7)))(kc, bk, c0),
                            reads=[("wB", kc), ("hT", c)], writes=[PS(bk)])
                P.op("act", lambda e: e.copy(out=vmt[b2][:, :, 0:128], in_=vst[:, c, :].rearrange("p (a b) -> p a b", a=4)),
                     reads=[("vst", c)], writes=[("vmt", b2)])
                og = ogt[b2]
                P.op("act", lambda e: e.activation(out=og, in_=bank[7], func=AF.Exp, scale=-1.0), reads=[PS(7)],
                     writes=[("ogt", b2)])
                P.op("act", lambda e: e.activation(out=og, in_=og, func=AF.Ln, bias=1.0), reads=[("ogt", b2)],
                     writes=[("ogt", b2)])
                P.op("act", lambda e: e.activation(out=og, in_=og, func=AF.Exp, scale=-1.0), reads=[("ogt", b2)],
                     writes=[("ogt", b2)])
                P.op("pool", lambda e: e.tensor_tensor(out=og, in0=og, in1=gmb, op=ALU.mult),
                     reads=[("ogt", b2), ("gmb",)], writes=[("ogt", b2)])
                for h in range(4):
                    P.op("pe", (lambda h: lambda e: e.matmul(bank[0][:, h * 128:(h + 1) * 128], lhsT=qk[:, 4 + h, cs],
                                                             rhs=qk[:, h, cs], start=True, stop=True))(h),
                         reads=[("qk", h, tb), ("qk", 4 + h, tb)], writes=[PS(0)])
                for h in range(4):
                    P.op("dve", (lambda h: lambda e: e.scalar_tensor_tensor(
                        out=spT[b2][:, h * 128:(h + 1) * 128], in0=bank[0][:, h * 128:(h + 1) * 128],
                        scalar=tokS[:, c, h:h + 1], in1=maskM, op0=ALU.mult, op1=ALU.mult))(h),
                        reads=[PS(0), ("tokS", c // 8)] + CONST, writes=[("spT", b2, h)])
                for h in range(4):
                    P.op("pe", (lambda h: lambda e: e.transpose(out=bankb[3][:, h * 128:(h + 1) * 128], in_=qk[:, 4 + h, cs],
                                                                identity=ident_b))(h),
                         reads=[("qk", 4 + h, tb)] + CONST, writes=[PS(3)])
                for h in range(4):
                    P.op("act", (lambda h: lambda e: e.activation(out=K2[b2][:, h, :], in_=bankb[3][:, h * 128:(h + 1) * 128],
                                                                  func=AF.Copy, scale=tokS[:, c, 4 + h:5 + h]))(h),
                         reads=[PS(3), ("tokS", c // 8)], writes=[("K2", b2, h)])

            def W1b(c):
                b2 = c % 2
                for h in range(4):
                    pkv = bank[4 + h // 2][:, (h % 2) * 129:(h % 2) * 129 + 129]
                    P.op("pe", (lambda h, pkv: lambda e: e.matmul(pkv, lhsT=K2[b2][:, h, :], rhs=vmt[b2][:, h, :], start=True,
                                                                  stop=True))(h, pkv),
                         reads=[("K2", b2, h), ("vmt", b2), ("vmt", "ones", b2)], writes=[PS(4 + h // 2)])

            def W2a(c):
                tb = c // 4
                cs = slice(c * 128, (c + 1) * 128)
                b2 = c % 2
                for h in range(4):
                    pn = bank[1 + h // 2][:, (h % 2) * 129:(h % 2) * 129 + 129]
                    P.op("pe", (lambda h, pn: lambda e: e.matmul(pn, lhsT=spT[b2][:, h * 128:(h + 1) * 128], rhs=vmt[b2][:, h, :],
                                                                 start=True, stop=False))(h, pn),
                         reads=[("spT", b2, h), ("vmt", b2), ("vmt", "ones", b2)], writes=[PS(1 + h // 2)])
                    P.op("pe", (lambda h, pn: lambda e: e.matmul(pn, lhsT=qk[:, h, cs], rhs=Cb[:, h, :], start=False,
                                                                 stop=True))(h, pn),
                         reads=[("qk", h, tb), ("Cb",)], writes=[PS(1 + h // 2)])
                for h in range(4):
                    pkv = bank[4 + h // 2][:, (h % 2) * 129:(h % 2) * 129 + 129]
                    P.op("dve", (lambda h, pkv: lambda e: e.scalar_tensor_tensor(
                        out=Cf[:, h, :], in0=Cf[:, h, :], scalar=decb[:, h, c:c + 1], in1=pkv, op0=ALU.mult,
                        op1=ALU.add))(h, pkv),
                        reads=[PS(4 + h // 2), ("decb",), ("Cf",)], writes=[("Cf",)])
                P.op("act", lambda e: e.copy(out=Cb, in_=Cf), reads=[("Cf",)], writes=[("Cb",)])

            def W2b(c):
                cs = slice(c * 128, (c + 1) * 128)
                b2 = c % 2
                og = ogt[b2]
                for hp in range(2):
                    den = bank[1 + hp][:, 0:258].rearrange("p (a b) -> p a b", a=2)[:, :, 128]
                    P.op("dve", (lambda hp, den: lambda e: e.tensor_scalar(
                        out=sm[:, 32 + 2 * hp:34 + 2 * hp], in0=den, scalar1=-1.0, scalar2=None, op0=ALU.mult))(hp, den),
                        reads=[PS(1 + hp)], writes=[("sm", "nd", hp)])
                    P.op("dve", (lambda hp, den: lambda e: e.tensor_tensor(
                        out=sm[:, 32 + 2 * hp:34 + 2 * hp], in0=den, in1=sm[:, 32 + 2 * hp:34 + 2 * hp], op=ALU.max))(hp, den),
                        reads=[PS(1 + hp), ("sm", "nd", hp)], writes=[("sm", "nd", hp)])
                P.op("dve", lambda e: e.tensor_tensor(out=sm[:, 0:4], in0=sm[:, 32:36], in1=tokS[:, c, 8:12], op=ALU.max),
                     reads=[("sm", "nd", 0), ("sm", "nd", 1), ("tokS", c // 8)], writes=[("sm", "m")])
                P.op("dve", lambda e: e.reciprocal(out=sm[:, 4:8], in_=sm[:, 0:4]), reads=[("sm", "m")], writes=[("sm", "r")])
                for h in range(4):
                    pn = bank[1 + h // 2][:, (h % 2) * 129:(h % 2) * 129 + 128]
                    P.op("act", (lambda h, pn: lambda e: e.activation(out=hmt[:, h * 128:(h + 1) * 128], in_=pn,
                                                                      func=AF.Square, accum_out=sm[:, 8 + h:9 + h]))(h, pn),
                         reads=[PS(1 + h // 2)], writes=[("hmt", h), ("sm", "ss", h)])
                P.op("dve", lambda e: e.tensor_tensor(out=sm[:, 12:16], in0=sm[:, 4:8], in1=sm[:, 4:8], op=ALU.mult),
                     reads=[("sm", "r")], writes=[("sm", "t")])
                P.op("dve", lambda e: e.tensor_tensor(out=sm[:, 12:16], in0=sm[:, 12:16], in1=sm[:, 8:12], op=ALU.mult),
                     reads=[("sm", "t")] + [("sm", "ss", h) for h in range(4)], writes=[("sm", "t")])
                P.op("act", lambda e: e.activation(out=sm[:, 16:20], in_=sm[:, 12:16], func=AF.Ln, scale=1.0 / 128, bias=EPS),
                     reads=[("sm", "t")], writes=[("sm", "ln")])
                P.op("act", lambda e: e.activation(out=sm[:, 20:24], in_=sm[:, 16:20], func=AF.Exp, scale=-0.5),
                     reads=[("sm", "ln")], writes=[("sm", "rs")])
                P.op("dve", lambda e: e.tensor_tensor(out=sm[:, 24:28], in0=sm[:, 20:24], in1=sm[:, 4:8], op=ALU.mult),
                     reads=[("sm", "rs"), ("sm", "r")], writes=[("sm", "fac")])
                for h in range(4):
                    pn = bank[1 + h // 2][:, (h % 2) * 129:(h % 2) * 129 + 128]
                    P.op("dve", (lambda h, pn: lambda e: e.scalar_tensor_tensor(
                        out=hmt[:, h * 128:(h + 1) * 128], in0=pn, scalar=sm[:, 24 + h:25 + h],
                        in1=og[:, h * 128:(h + 1) * 128], op0=ALU.mult, op1=ALU.mult))(h, pn),
                        reads=[PS(1 + h // 2), ("sm", "fac"), ("ogt", b2)], writes=[("hmt", h)])
                for h in range(4):
                    P.op("pe", (lambda h: lambda e: e.transpose(out=bankb[3][:, 512 + h * 128:512 + (h + 1) * 128],
                                                                in_=hmt[:, h * 128:(h + 1) * 128], identity=ident_b))(h),
                         reads=[("hmt", h)] + CONST, writes=[PS(3)])
                P.op("act", lambda e: e.copy(out=mixT[:, 0:4, cs],
                                             in_=bankb[3][:, 512:1024].rearrange("p (a b) -> p a b", a=4)),
                     reads=[PS(3)], writes=[("mixT", "m", c)])

            W1a(0)
            W1b(0)
            for c in range(NT):
                if c + 1 < NT:
                    W1a(c + 1)
                W2a(c)
                if c + 1 < NT:
                    W1b(c + 1)
                W2b(c)

            load_w(wA, "wA", w_in_d, 2056, 1024)
            load_w(wB, "wB", w_in_d, 3080, 512)
            P.alias(["vs"], RAWGRP + ["rows"])
            P.alias(["mixT"], ["vst"])
            for cc in range(8):
                for tb in range(4):
                    bk = tb % 2
                    for kc in range(8):
                        P.op("pe", (lambda kc, cc, tb, bk: lambda e: e.matmul(
                            bank[bk], lhsT=wA[:, kc, cc * 128:(cc + 1) * 128], rhs=hT[:, kc, tb * 512:(tb + 1) * 512],
                            start=(kc == 0), stop=(kc == 7)))(kc, cc, tb, bk),
                            reads=[("wA", kc)] + [("hT", tb * 4 + i) for i in range(4)], writes=[PS(bk)])
                    eng = "act" if tb % 2 == 0 else "dve"
                    if eng == "act":
                        P.op("act", (lambda cc, tb, bk: lambda e: e.copy(out=qk[:, cc, tb * 512:(tb + 1) * 512],
                                                                         in_=bank[bk]))(cc, tb, bk),
                             reads=[PS(bk)], writes=[("qk", cc, tb)])
                    else:
                        P.op("dve", (lambda cc, tb, bk: lambda e: e.tensor_copy(out=qk[:, cc, tb * 512:(tb + 1) * 512],
                                                                                in_=bank[bk]))(cc, tb, bk),
                             reads=[PS(bk)], writes=[("qk", cc, tb)])
            for tt in range(NT):
                bk = 2 + tt % 2
                for kc in range(8):
                    P.op("pe", (lambda kc, tt, bk: lambda e: e.matmul(
                        bank[bk], lhsT=hT[:, kc, tt * 128:(tt + 1) * 128], rhs=wB[:, kc, 0:512], start=(kc == 0),
                        stop=(kc == 7)))(kc, tt, bk),
                        reads=[("wB", kc), ("hT", tt)], writes=[PS(bk)])
                P.op("act" if tt % 2 else "dve",
                     (lambda tt, bk: (lambda e: e.copy(out=vs[:, tt, :], in_=bank[bk])) if tt % 2 else
                      (lambda e: e.tensor_copy(out=vs[:, tt, :], in_=bank[bk])))(tt, bk),
                     reads=[PS(bk)], writes=[("vs", tt)])

            load_w(wA, "wA", w_out_d, 0, 1024)
            P.alias(ATTGRP, ["wB"])
            units = []
            for h in range(8):
                for qt in range(4):
                    nblk = 4 * qt + 4
                    for bi, sb in enumerate(range(nblk - 1, -1, -1)):
                        i = sb - 4 * qt
                        units.append(dict(h=h, qt=qt, sb=sb, bi=bi, first=(bi == 0), last=(sb == 0), diag=(i >= 0),
                                          c0=(128 * i if i > 0 else 0)))
            NU = len(units)
            for k, u in enumerate(units):
                u["zb"] = k % 3
                u["e3"] = k % 3
                u["e2"] = k % 2
                u["pr"] = (u["h"] % 2) * 64
                u["qc"] = u["h"] // 2
                u["kc"] = 4 + u["h"] // 2
                u["pb"] = 3 + u["h"] % 2
                u["ob"] = 5 + u["qt"] % 2
                u["cols"] = slice(u["c0"], 512)

            def st_Z(u):
                zb, sb, cols, pr, qc, kc_, t0, c0 = u["zb"], u["sb"], u["cols"], u["pr"], u["qc"], u["kc"], u["qt"] * 512, u["c0"]
                diag = u["diag"]
                P.op("pe", lambda e: e.matmul(bank[zb][:, cols], lhsT=qk[pr:pr + 64, kc_, sb * 128:(sb + 1) * 128],
                                              rhs=qk[pr:pr + 64, qc, t0 + c0:t0 + 512], start=True, stop=not diag,
                                              skip_group_check=True),
                     reads=[("qk", qc, u["qt"]), ("qk", kc_, sb // 4)], writes=[PS(zb)])
                if diag:
                    P.op("pe", lambda e: e.matmul(bank[zb][:, c0:c0 + 128], lhsT=ident_b, rhs=negm, start=False, stop=True,
                                                  skip_group_check=True),
                         reads=CONST, writes=[PS(zb)])

            def st_E_SP(u):
                zb, cols, e3 = u["zb"], u["cols"], u["e3"]
                E, SP = Ebuf[e3], SPbuf[e3]
                P.op("act", lambda e: e.activation(out=E[:, cols], in_=bank[zb][:, cols], func=AF.Exp, scale=0.125),
                     reads=[PS(zb)], writes=[("E", e3)])
                P.op("act", lambda e: e.activation(out=SP[:, cols], in_=E[:, cols], func=AF.Ln, bias=1.0),
                     reads=[("E", e3)], writes=[("SP", e3)])

            def st_mm1(u):
                pb, cols, e3, first = u["pb"], u["cols"], u["e3"], u["first"]
                SP = SPbuf[e3]
                P.op("pe", lambda e: e.matmul(bank[pb][:, cols], lhsT=triU, rhs=SP[:, cols], start=first, stop=False,
                                              skip_group_check=True),
                     reads=[("SP", e3)] + CONST, writes=[PS(pb)])

            def st_mm2(u):
                if u["last"]:
                    return
                pb, cols, e3 = u["pb"], u["cols"], u["e3"]
                SP = SPbuf[e3]
                P.op("pe", lambda e: e.matmul(bank[pb][:, cols], lhsT=comp, rhs=SP[:, cols], start=False, stop=False,
                                              skip_group_check=True),
                     reads=[("SP", e3)] + CONST, writes=[PS(pb)])

            def st_ER(u):
                pb, cols, e2 = u["pb"], u["cols"], u["e2"]
                ER = ERbuf[e2]
                P.op("act", lambda e: e.activation(out=ER[:, cols], in_=bank[pb][:, cols], func=AF.Exp, scale=-1.0),
                     reads=[PS(pb)], writes=[("ER", e2)])

            def st_AT(u):
                cols, e3, e2 = u["cols"], u["e3"], u["e2"]
                E, ER, AT = Ebuf[e3], ERbuf[e2], ATbuf[e3]
                P.op("dve", lambda e: e.tensor_tensor(out=AT[:, cols], in0=E[:, cols], in1=ER[:, cols], op=ALU.mult),
                     reads=[("E", e3), ("ER", e2)], writes=[("AT", e3)])

            def st_AV(u):
                cols, e3, sb, h, pr, ob, first, last = u["cols"], u["e3"], u["sb"], u["h"], u["pr"], u["ob"], u["first"], u["last"]
                AT = ATbuf[e3]
                hp0 = (h // 2) * 128
                P.op("pe", lambda e: e.matmul(bank[ob][:, cols], lhsT=vs[:, sb, hp0:hp0 + 128], rhs=AT[:, cols],
                                              start=first, stop=last, skip_group_check=True),
                     reads=[("AT", e3), ("vs", sb)], writes=[PS(ob)])
                for _d in range(NDUMMY):
                    P.op("pe", lambda e: e.matmul(bank[7], lhsT=triU, rhs=ATbuf[e3][:, 0:512], start=True, stop=True,
                                                  skip_group_check=True),
                         reads=CONST, writes=[PS(7)])
                if last:
                    qc, t0, qt = u["qc"], u["qt"] * 512, u["qt"]
                    P.op("dve", lambda e: e.tensor_copy(out=mixT[pr:pr + 64, 4 + qc, t0:t0 + 512], in_=bank[ob][pr:pr + 64, :]),
                         reads=[PS(ob)], writes=[("mixT", "s", h, qt)])

            st_Z(units[0])
            st_Z(units[1])
            st_E_SP(units[0])
            for k in range(NU):
                if k >= 1:
                    st_mm2(units[k - 1])
                st_mm1(units[k])
                if k + 2 < NU:
                    st_Z(units[k + 2])
                if k >= 1:
                    st_AV(units[k - 1])
                if k + 1 < NU:
                    st_E_SP(units[k + 1])
                st_ER(units[k])
                st_AT(units[k])
                if sq + 1 < nseq and k % 20 == 10:
                    tt = k // 20
                    norm_transpose(x_d[sq + 1, tt * 128:(tt + 1) * 128, :], [], tt % 2, g1T, ("g1T",), hT, ("hT", tt), tt * 128)
            st_AV(units[NU - 1])

            if dbg_d is not None:
                P.op("sp", (lambda sq: lambda e: e.dma_start(out=dbg_d[sq], in_=mixT.rearrange("p a b -> p (a b)")))(sq),
                     reads=[("mixT", "m", c) for c in range(NT)] + [("mixT", "s", h, qt) for h in range(8) for qt in range(4)],
                     dma=True)

            for tt in range(NT):
                b = tt % 2
                yb = 2 * b
                xb4 = tt % 4
                P.op("sp", (lambda sq, tt, xb4: lambda e: e.dma_start(out=xt[xb4], in_=x_d[sq, tt * 128:(tt + 1) * 128, :]))(sq, tt, xb4),
                     writes=[("xt", xb4)], dma=True)
                mt = [("mixT", "m", tt)] + [("mixT", "s", h, tt // 4) for h in range(8)]
                for half in range(2):
                    for kc in range(8):
                        P.op("pe", (lambda kc, tt, half, yb: lambda e: e.matmul(
                            bank[yb + half], lhsT=mixT[:, kc, tt * 128:(tt + 1) * 128],
                            rhs=wA[:, kc, half * 512:(half + 1) * 512], start=(kc == 0), stop=(kc == 7)))(kc, tt, half, yb),
                            reads=[("wA", kc)] + mt, writes=[PS(yb + half)])
                me = post_norm_residual(yb, b, g2b, ("g2b",), [], out_d[sq, tt * 128:(tt + 1) * 128, :], [("out", sq, tt)],
                                        xb=xb4)
                if not do_ffn:
                    fin.append(me)

        if do_ffn:
            R1NAMES = ["hT", "qk", "raw0", "raw1", "diagM", "vs", "rows", "wB", "E", "ER", "SP", "AT"]
            R2NAMES = ["mixT", "vst", "wA"]
            R3NAMES = ["g1T", "g2b", "gmb", "cwm", "cbm", "bif", "ifr", "tokS", "Cf", "Cb", "K2", "spT", "hmt", "vmt", "ogt",
                       "decb", "sm", "rsm"]
            A.off = region1_at
            wup, _ = A.take([128, 8, 2 * DFF], BF16)
            h2T, _ = A.take([128, 8, FB], BF16)
            assert A.off <= region1_end, (A.off, region1_end)
            A.off = region1_end
            wdn, _ = A.take([128, NFF, D], BF16)
            g3T, _ = A.take([128, 1024], F32)
            assert A.off <= region2_end, (A.off, region2_end)
            A.off = region2_end
            g4b, _ = A.take([128, 1024], F32)
            cwf, _ = A.take([128, NFF * 3], F32)
            cbf, _ = A.take([128, NFF], F32)
            actT, _ = A.take([128, NFF, FB], BF16)
            graw = [A.take([128, 2 + FB], F32)[0] for _ in range(2)]
            cv = [A.take([128, FB], F32)[0] for _ in range(2)]
            gel = [A.take([128, FB], BF16)[0] for _ in range(2)]
            halo, _ = A.take([128, NFF, 2], F32)
            P.alias(["wupk0", "wupk1"], ["hT"])
            P.alias(["wupk%d" % k for k in range(2, 8)] + ["h2T"], R1NAMES)
            P.alias(["wdn", "g3T"], R2NAMES)
            P.alias(["g4b", "cwf", "cbf", "actT", "graw", "cv", "gel", "halo"], R3NAMES)
            for (ap, d_ap, nm) in [(g3T, g3T_d, "g3T"), (g4b, g4b_d, "g4b"), (cwf, cwf_d, "cwf"), (cbf, cbf_d, "cbf")]:
                P.op("sp", (lambda ap, d_ap: lambda e: e.dma_start(out=ap, in_=d_ap))(ap, d_ap), writes=[(nm,)], dma=True)
            HC = (NFF // 2) * 128
            assert 2 * (2 * DFF) * 2 <= 8 * S * 2
            for kcs in ((0, 1), (2, 3, 4, 5, 6, 7)):
                for sp_ in range(2):
                    for (nm, base) in (("g", 0), ("u", DFF)):
                        c0 = base + sp_ * HC
                        for kc in kcs:
                            P.op("pool", (lambda c0, kc: lambda e: e.dma_start(
                                out=wup[:, kc, c0:c0 + HC], in_=w_up_d[kc * 128:(kc + 1) * 128, c0:c0 + HC]))(c0, kc),
                                writes=[("wupk%d" % kc, nm, sp_)], dma=True)
            for j in range(NFF):
                P.op("pool", (lambda j: lambda e: e.dma_start(out=wdn[:, j, :], in_=w_dn_d[j * 128:(j + 1) * 128, :]))(j),
                     writes=[("wdn", j)], dma=True)
            nblk_seq = S // FB
            blocks = [(sq, blk) for sq in range(nseq) for blk in range(nblk_seq)]
            NB = len(blocks)

            TPB = FB // 128

            def f_norm_a(i, tl):
                sq, blk = blocks[i]
                tt = blk * TPB + tl
                xb = tl % 2
                P.op("sp", lambda e: e.dma_start(out=xt[xb], in_=out_d[sq, tt * 128:(tt + 1) * 128, :]),
                     reads=[("out", sq, tt)], writes=[("xt", xb)], dma=True)
                rstd, rtok = rms_rstd(xt[xb], [("xt", xb)], D, 4 * xb, xh[xb], ("xh", xb))
                P.op("dve", lambda e: e.tensor_scalar(out=xh[xb], in0=xt[xb], scalar1=rstd, scalar2=None, op0=ALU.mult),
                     reads=[("xt", xb), rtok], writes=[("xh", xb)])

            def f_norm_b(i, tl):
                xb = tl % 2
                for c in range(8):
                    P.op("pe", (lambda c: lambda e: e.transpose(out=bankb[0][:, c * 128:(c + 1) * 128],
                                                                in_=xh[xb][:, c * 128:(c + 1) * 128], identity=ident_b))(c),
                         reads=[("xh", xb)] + CONST, writes=[PS(0)])
                P.op("dve", lambda e: e.tensor_tensor(
                    out=h2T[:, :, tl * 128:(tl + 1) * 128], in0=bankb[0].rearrange("p (c t) -> p c t", c=8),
                    in1=g3T.rearrange("p (c t) -> p c t", c=8), op=ALU.mult),
                    reads=[PS(0), ("g3T",)], writes=[("h2T", tl)])

            def f_up(i, mid_hook):
                sq, blk = blocks[i]
                H2 = [("h2T", tl) for tl in range(TPB)]
                if blk == 0:
                    P.op("pool", lambda e: e.memset(halo, 0.0), writes=[("halo", j) for j in range(NFF)])
                for j in range(NFF):
                    gb = j % 2
                    for kc in range(8):
                        P.op("pe", (lambda kc, j, gb: lambda e: e.matmul(
                            bank[gb][:, 0:FB], lhsT=wup[:, kc, j * 128:(j + 1) * 128], rhs=h2T[:, kc, :],
                            start=(kc == 0), stop=(kc == 7)))(kc, j, gb),
                            reads=[("wupk%d" % kc, "g", j // (NFF // 2))] + H2, writes=[PS(gb)])
                    for kc in range(8):
                        P.op("pe", (lambda kc, j, gb: lambda e: e.matmul(
                            bank[2 + gb][:, 0:FB], lhsT=wup[:, kc, DFF + j * 128:DFF + (j + 1) * 128], rhs=h2T[:, kc, :],
                            start=(kc == 0), stop=(kc == 7)))(kc, j, gb),
                            reads=[("wupk%d" % kc, "u", j // (NFF // 2))] + H2, writes=[PS(2 + gb)])
                    P.op("pool", (lambda j, gb: lambda e: e.tensor_copy(out=graw[gb][:, 0:2], in_=halo[:, j, :]))(j, gb),
                         reads=[("halo", j)], writes=[("graw", gb, "h")])
                    P.op("act", (lambda gb: lambda e: e.copy(out=graw[gb][:, 2:2 + FB], in_=bank[gb][:, 0:FB]))(gb),
                         reads=[PS(gb)], writes=[("graw", gb)])
                    P.op("pool", (lambda j, gb: lambda e: e.tensor_copy(out=halo[:, j, :], in_=graw[gb][:, FB:FB + 2]))(j, gb),
                         reads=[("graw", gb)], writes=[("halo", j)])
                    GR = [("graw", gb), ("graw", gb, "h")]
                    P.op("dve", (lambda j, gb: lambda e: e.tensor_scalar(out=cv[gb], in0=graw[gb][:, 2:2 + FB],
                                                                         scalar1=cwf[:, j * 3 + 2:j * 3 + 3], scalar2=None,
                                                                         op0=ALU.mult))(j, gb),
                         reads=GR + [("cwf",)], writes=[("cv", gb)])
                    for tp in (1, 0):
                        P.op("dve", (lambda j, gb, tp: lambda e: e.scalar_tensor_tensor(
                            out=cv[gb], in0=graw[gb][:, tp:tp + FB], scalar=cwf[:, j * 3 + tp:j * 3 + tp + 1], in1=cv[gb],
                            op0=ALU.mult, op1=ALU.add))(j, gb, tp),
                            reads=GR + [("cwf",), ("cv", gb)], writes=[("cv", gb)])
                    P.op("act", (lambda j, gb: lambda e: e.activation(out=gel[gb], in_=cv[gb], func=AF.Gelu_apprx_tanh,
                                                                      bias=cbf[:, j:j + 1]))(j, gb),
                         reads=[("cv", gb), ("cbf",)], writes=[("gel", gb)])
                    P.op("dve", (lambda j, gb: lambda e: e.tensor_tensor(out=actT[:, j, :], in0=bank[2 + gb][:, 0:FB],
                                                                         in1=gel[gb], op=ALU.mult))(j, gb),
                         reads=[PS(2 + gb), ("gel", gb)], writes=[("actT", j)])
                    if j == 9:
                        mid_hook()

            def f_down(i, tl):
                sq, blk = blocks[i]
                AT_ALL = [("actT", j) for j in range(NFF)]
                tt = blk * TPB + tl
                yb = 4 + 2 * (tl % 2)
                xb = 2 + tl % 2
                P.op("sp", lambda e: e.dma_start(out=xt[xb], in_=out_d[sq, tt * 128:(tt + 1) * 128, :]),
                     reads=[("out", sq, tt)], writes=[("xt", xb)], dma=True)
                for half in range(2):
                    for j in range(NFF):
                        P.op("pe", (lambda j, half: lambda e: e.matmul(
                            bank[yb + half], lhsT=actT[:, j, tl * 128:(tl + 1) * 128],
                            rhs=wdn[:, j, half * 512:(half + 1) * 512], start=(j == 0), stop=(j == NFF - 1)))(j, half),
                            reads=[("wdn", j)] + AT_ALL, writes=[PS(yb + half)])
                me = post_norm_residual(yb, tl % 2, g4b, ("g4b",), [], out_d[sq, tt * 128:(tt + 1) * 128, :],
                                        [("out", sq, tt)], xb=xb, junk=(cv[tl % 2].bitcast(BF16), ("cv", tl % 2)))
                fin.append(me)

            for tl in range(TPB):
                f_norm_a(0, tl)
                f_norm_b(0, tl)
            for i in range(NB):
                nxt = i + 1 < NB

                def mid(i=i, nxt=nxt):
                    if nxt:
                        f_norm_a(i + 1, 0)
                        f_norm_a(i + 1, 1)
                f_up(i, mid)
                for tl in range(TPB):
                    f_down(i, tl)
                    if nxt:
                        f_norm_b(i + 1, tl)
                        if tl + 2 < TPB:
                            f_norm_a(i + 1, tl + 2)
        print("SBUF arena high-water (KB):", A.hi / 1024.0, " ops:", {e: len(P.ops[e]) for e in P.ENGS})
        P.emit(final_waits=fin)
    return nc


_CACHE = {}


def _prep_small(inputs):
    f = lambda a: np.ascontiguousarray(np.asarray(a, dtype=np.float32))
    gT = lambda g: f(np.broadcast_to(g.reshape(8, 128).T[:, :, None], (128, 8, 128)).reshape(128, 1024))
    gb = lambda g: f(np.broadcast_to(g.reshape(1, -1), (128, g.size)))
    d = {}
    d["g1T"] = gT(np.asarray(inputs["pre_mix_norm"])[0])
    d["g3T"] = gT(np.asarray(inputs["pre_ffn_norm"])[0])
    d["g2b"] = gb(np.asarray(inputs["post_mix_norm"])[0])
    d["g4b"] = gb(np.asarray(inputs["post_ffn_norm"])[0])
    d["gmb"] = gb(np.asarray(inputs["mlstm_norm"])[0])
    cw = np.asarray(inputs["mlstm_conv_w"])[0]
    d["cwm"] = f(cw.reshape(4, 8, 128).transpose(2, 1, 0).reshape(128, 32))
    d["cbm"] = f(np.asarray(inputs["mlstm_conv_b"])[0].reshape(8, 128).T)
    d["bif"] = f(np.stack([np.asarray(inputs["mlstm_b_i"])[0], np.asarray(inputs["mlstm_b_f"])[0]], axis=1))
    fw = np.asarray(inputs["ffn_conv_w"])[0]
    d["cwf"] = f(fw.reshape(3, NFF, 128).transpose(2, 1, 0).reshape(128, NFF * 3))
    d["cbf"] = f(np.asarray(inputs["ffn_conv_b"])[0].reshape(NFF, 128).T)
    return d


def kernel(**inputs):
    x = np.asarray(inputs["x"], dtype=np.float32)
    nseq = x.shape[0] // NCORES
    if "nc" not in _CACHE:
        _CACHE["nc"] = build_program(nseq=nseq)
    nc = _CACHE["nc"]
    small = _prep_small(inputs)
    shared = {
        "w_in": np.ascontiguousarray(np.asarray(inputs["w_in"], dtype=np.float32)[0]),
        "w_out": np.ascontiguousarray(np.asarray(inputs["w_out"], dtype=np.float32)[0]),
        "w_up": np.ascontiguousarray(np.asarray(inputs["w_up"], dtype=np.float32)[0]),
        "w_down": np.ascontiguousarray(np.asarray(inputs["w_down"], dtype=np.float32)[0]),
    }
    shared.update(small)
    in_maps = []
    for c in range(NCORES):
        m = dict(shared)
        m["x"] = np.ascontiguousarray(x[c * nseq:(c + 1) * nseq])
        in_maps.append(m)
    res = run_bass_kernel_spmd(nc, in_maps, core_ids=list(range(NCORES)))
    return np.concatenate([np.asarray(r["out"]) for r in res.results], axis=0).astype(np.float32)
```

```python
import contextlib
import math
import numpy as np
import concourse.bass as bass
import concourse.mybir as mybir
from concourse.bass_utils import run_bass_kernel_spmd

F32 = mybir.dt.float32
BF16 = mybir.dt.bfloat16
U8 = mybir.dt.uint8
AF = mybir.ActivationFunctionType
ALU = mybir.AluOpType

NCORES = 8
S = 2048
D = 1024
NT = S // 128
DFF = 2816
NFF = DFF // 128
INC = 3592
EPS = 1e-6
FB = 512
NDUMMY = 2


class Prog:
    ENGS = ("pe", "act", "dve", "pool", "sp")
    NDMA = 48

    def __init__(self, nc):
        self.nc = nc
        self.ops = {e: [] for e in self.ENGS}
        self.last_w = {}
        self.readers = {}
        self.ndma = 0
        self.alias_of = {}

    def alias(self, new_names, old_names):
        deps = set()
        old = set(old_names)
        for t, w in self.last_w.items():
            if t[0] in old and w is not None:
                deps.add(w)
        for t, rs in self.readers.items():
            if t[0] in old:
                deps.update(rs)
        deps = list(deps)
        new = set(new_names)
        for t in self.readers:
            if t[0] in new:
                self.readers[t] = self.readers[t] + deps
        for n in new_names:
            self.alias_of[n] = deps

    def _touch(self, t):
        if t not in self.last_w and t not in self.readers:
            self.last_w[t] = None
            self.readers[t] = list(self.alias_of.get(t[0], ()))

    def _dep(self, o, prod):
        if prod is None:
            return
        peng, pidx, pdma = prod
        if pdma is None and peng == o["eng"] and (peng == "pe" or pidx == o["idx"]):
            return
        o["waits"].add(prod)

    def op(self, eng, fn, reads=(), writes=(), dma=False):
        idx = len(self.ops[eng])
        o = {"eng": eng, "fn": fn, "waits": set(), "dma": None, "idx": idx, "inc": False}
        if dma:
            o["dma"] = self.ndma
            self.ndma += 1
        me = (eng, idx, o["dma"])
        writes = list(writes) + [t for t in reads if t[0] == "ps"]
        reads = [t for t in reads if t[0] != "ps"]
        for t in list(reads) + list(writes):
            self._touch(t)
        for t in reads:
            self._dep(o, self.last_w.get(t))
        for t in writes:
            self._dep(o, self.last_w.get(t))
            for r in self.readers.get(t, ()):
                if r != me:
                    self._dep(o, r)
        for t in reads:
            self.readers[t].append(me)
        for t in writes:
            self.last_w[t] = me
            self.readers[t] = []
        self.ops[eng].append(o)
        return me

    def emit(self, final_waits=()):
        nc = self.nc
        for e in self.ENGS:
            for o in self.ops[e]:
                for (pe, pi, pd) in o["waits"]:
                    if pd is None:
                        self.ops[pe][pi]["inc"] = True
        semval = {}
        for e in self.ENGS:
            c = 0
            for o in self.ops[e]:
                if o["dma"] is None and o["inc"]:
                    c += 1
                    semval[(e, o["idx"])] = c
        dma_slot, dma_val, dma_prev = {}, {}, {}
        slot_cnt = [0] * self.NDMA
        slot_last = [None] * self.NDMA
        order = [o for e in self.ENGS for o in self.ops[e] if o["dma"] is not None]
        order.sort(key=lambda o: o["dma"])
        half = self.NDMA // 2
        rr = {"hw": 0, "sw": 0}
        for o in order:
            if o["eng"] == "pool":
                s = half + rr["sw"] % half
                rr["sw"] += 1
            else:
                s = rr["hw"] % half
                rr["hw"] += 1
            slot_cnt[s] += 1
            dma_slot[o["dma"]] = s
            dma_val[o["dma"]] = 16 * slot_cnt[s]
            dma_prev[o["dma"]] = slot_last[s]
            slot_last[s] = o["dma"]

        with contextlib.ExitStack() as st:
            esem = {e: st.enter_context(nc.semaphore("s_" + e)) for e in ("pe", "act", "dve", "pool")}
            dsem = [st.enter_context(nc.semaphore("d%d" % i)) for i in range(self.NDMA)]
            block = st.enter_context(nc.Block())

            def run(ename, eng):
                seen = {}
                for o in self.ops[ename]:
                    waits = []
                    for (pe, pi, pd) in o["waits"]:
                        if pd is None:
                            waits.append((("e", pe), esem[pe], semval[(pe, pi)]))
                        else:
                            waits.append((("d", dma_slot[pd]), dsem[dma_slot[pd]], dma_val[pd]))
                    if o["dma"] is not None and dma_prev[o["dma"]] is not None:
                        pd = dma_prev[o["dma"]]
                        waits.append((("d", dma_slot[pd]), dsem[dma_slot[pd]], dma_val[pd]))
                    best = {}
                    for k, s, v in waits:
                        if seen.get(k, 0) >= v:
                            continue
                        if k not in best or best[k][1] < v:
                            best[k] = (s, v)
                    for k, (s, v) in best.items():
                        eng.wait_ge(s, v)
                        seen[k] = v
                    ins = o["fn"](eng)
                    if o["dma"] is not None:
                        ins.then_inc(dsem[dma_slot[o["dma"]]], 16)
                    elif o["inc"]:
                        ins.then_inc(esem[ename], 1)
                if ename == "sp":
                    for (pe, pi, pd) in final_waits:
                        eng.wait_ge(dsem[dma_slot[pd]], dma_val[pd])

            block.tensor(lambda eng: run("pe", eng))
            block.scalar(lambda eng: run("act", eng))
            block.vector(lambda eng: run("dve", eng))
            block.gpsimd(lambda eng: run("pool", eng))
            block.sync(lambda eng: run("sp", eng))


class Arena:
    def __init__(self, tensor, size):
        self.t = tensor
        self.size = size
        self.off = 0
        self.hi = 0

    def take(self, shape, dt, at=None):
        esz = {F32: 4, BF16: 2}[dt]
        n = int(np.prod(shape[1:])) * esz
        n_al = (n + 63) // 64 * 64
        if at is None:
            at = self.off
            self.off += n_al
        self.hi = max(self.hi, at + n_al)
        assert at + n_al <= self.size, ("SBUF arena overflow", at + n_al, self.size)
        v = self.t[:, at:at + n].bitcast(dt)
        if len(shape) == 3:
            v = v.rearrange("p (a b) -> p a b", a=shape[1])
        elif len(shape) == 4:
            v = v.rearrange("p (a b c) -> p a b c", a=shape[1], b=shape[2])
        return v, at


def build_program(nseq=2, do_ffn=True, debug_mix=False):
    nc = bass.Bass("TRN2", target_bir_lowering=False)
    dr = lambda name, shape, dt=F32, kind="ExternalInput": nc.dram_tensor(name, shape, dt, kind=kind).ap()
    x_d = dr("x", [nseq, S, D])
    w_in_d = dr("w_in", [D, INC])
    w_out_d = dr("w_out", [D, D])
    w_up_d = dr("w_up", [D, 2 * DFF])
    w_dn_d = dr("w_down", [DFF, D])
    g1T_d = dr("g1T", [128, 1024])
    g3T_d = dr("g3T", [128, 1024])
    g2b_d = dr("g2b", [128, 1024])
    g4b_d = dr("g4b", [128, 1024])
    gmb_d = dr("gmb", [128, 512])
    cwm_d = dr("cwm", [128, 32])
    cbm_d = dr("cbm", [128, 8])
    bif_d = dr("bif", [4, 2])
    cwf_d = dr("cwf", [128, NFF * 3])
    cbf_d = dr("cbf", [128, NFF])
    out_d = dr("out", [nseq, S, D], kind="ExternalOutput")
    dbg_d = dr("dbg", [nseq, 128, 8 * S], BF16, kind="ExternalOutput") if debug_mix else None

    P = Prog(nc)
    with contextlib.ExitStack() as st:
        ARENA = 206 * 1024
        arena_t = st.enter_context(nc.sbuf_tensor("arena", [128, ARENA], U8))
        A = Arena(arena_t, ARENA)
        psum_t = st.enter_context(nc.psum_tensor("psum", [128, 4096], F32))
        bank = [psum_t[:, i * 512:(i + 1) * 512] for i in range(8)]
        bankb = [b.bitcast(BF16) for b in bank]

        def PS(i):
            return ("ps", i)

        ident_b, _ = A.take([128, 128], BF16)
        ident_f, _ = A.take([128, 128], F32)
        maskM, _ = A.take([128, 128], BF16)
        maskS, _ = A.take([128, 128], F32)
        triU, _ = A.take([128, 128], BF16)
        comp, _ = A.take([128, 128], BF16)
        negm, _ = A.take([128, 128], BF16)
        sel, _ = A.take([128, 4, 128], F32)
        xt = [A.take([128, 1024], F32)[0] for _ in range(4)]
        xh = [A.take([128, 1024], BF16)[0] for _ in range(2)]
        stat, _ = A.take([128, 16], F32)
        persist_end = A.off

        def const_tri(ap, pattern, cm, op, base=0, val=1.0):
            P.op("pool", lambda e: e.memset(ap, val), writes=[("const",)])
            P.op("pool", lambda e: e.affine_select(out=ap, in_=ap, pattern=pattern, compare_op=op, fill=0.0,
                                                   base=base, channel_multiplier=cm),
                 reads=[("const",)], writes=[("const",)])

        const_tri(ident_b, [[-1, 128]], 1, ALU.is_equal)
        const_tri(ident_f, [[-1, 128]], 1, ALU.is_equal)
        const_tri(maskM, [[1, 128]], -1, ALU.is_ge)
        const_tri(maskS, [[1, 128]], -1, ALU.is_gt)
        const_tri(triU, [[-1, 128]], 1, ALU.is_ge)
        const_tri(comp, [[1, 128]], -1, ALU.is_gt)
        const_tri(sel[0:4], [[-1, 4], [0, 128]], 1, ALU.is_equal)
        const_tri(negm, [[-1, 128]], 1, ALU.is_ge, val=-2400.0)
        CONST = [("const",)]

        def rms_rstd(src_ap, src_tok, n, col, junk_ap, junk_tok):
            P.op("act", lambda e: e.activation(out=junk_ap, in_=src_ap, func=AF.Square,
                                               accum_out=stat[:, col:col + 1]),
                 reads=src_tok, writes=[junk_tok, ("stat", col)])
            P.op("act", lambda e: e.activation(out=stat[:, col + 1:col + 2], in_=stat[:, col:col + 1], func=AF.Ln,
                                               scale=1.0 / n, bias=EPS),
                 reads=[("stat", col)], writes=[("stat", col + 1)])
            P.op("act", lambda e: e.activation(out=stat[:, col + 2:col + 3], in_=stat[:, col + 1:col + 2],
                                               func=AF.Exp, scale=-0.5),
                 reads=[("stat", col + 1)], writes=[("stat", col + 2)])
            return stat[:, col + 2:col + 3], ("stat", col + 2)

        def norm_transpose(src_dram_tile, src_tok, b, gT, gT_tok, dstT, dst_tok, tcol):
            P.op("sp", lambda e: e.dma_start(out=xt[b], in_=src_dram_tile), reads=src_tok, writes=[("xt", b)], dma=True)
            rstd, rtok = rms_rstd(xt[b], [("xt", b)], D, 4 * b, xh[b], ("xh", b))
            P.op("dve", lambda e: e.tensor_scalar(out=xh[b], in0=xt[b], scalar1=rstd, scalar2=None, op0=ALU.mult),
                 reads=[("xt", b), rtok], writes=[("xh", b)])
            for c in range(8):
                P.op("pe", (lambda c: lambda e: e.transpose(out=bankb[7][:, c * 128:(c + 1) * 128],
                                                            in_=xh[b][:, c * 128:(c + 1) * 128], identity=ident_b))(c),
                     reads=[("xh", b)] + CONST, writes=[PS(7)])
            P.op("dve", lambda e: e.tensor_tensor(out=dstT[:, :, tcol:tcol + 128],
                                                  in0=bankb[7].rearrange("p (c t) -> p c t", c=8),
                                                  in1=gT.rearrange("p (c t) -> p c t", c=8), op=ALU.mult),
                 reads=[PS(7), gT_tok], writes=[dst_tok])

        def post_norm_residual(ybanks, b, gb, gb_tok, resid_tok_reads, out_dram_tile, out_tok, xb=None, junk=None):
            xb = b if xb is None else xb
            yap = psum_t[:, ybanks * 512:ybanks * 512 + 1024]
            ytok = [PS(ybanks), PS(ybanks + 1)]
            jap, jtok = junk if junk is not None else (xh[b], ("xh", b))
            rstd, rtok = rms_rstd(yap, ytok, D, 8 + 4 * b, jap, jtok)
            P.op("dve", lambda e: e.scalar_tensor_tensor(out=yap, in0=yap, scalar=rstd, in1=gb, op0=ALU.mult, op1=ALU.mult),
                 reads=ytok + [rtok, gb_tok], writes=[])
            P.op("dve", lambda e: e.tensor_tensor(out=xt[xb], in0=yap, in1=xt[xb], op=ALU.add),
                 reads=ytok + [("xt", xb)] + list(resid_tok_reads), writes=[("xt", xb)])
            return P.op("sp", lambda e: e.dma_start(out=out_dram_tile, in_=xt[xb]), reads=[("xt", xb)],
                        writes=out_tok, dma=True)

        persist_end = A.off

        A.off = persist_end
        region1_at = A.off
        hT, _ = A.take([128, 8, S], BF16)
        qk, _ = A.take([128, 8, S], BF16)
        raw0, raw_at = A.take([128, 3 + S], BF16)
        raw1, _ = A.take([128, 3 + S], BF16)
        diagM, _ = A.take([128, 8, 4, 128], BF16)
        rawdiag_end = A.off
        wB, wB_at = A.take([128, 8, 1032], BF16)
        region1_end = A.off
        mixT, mixT_at = A.take([128, 8, S], BF16)
        wA, _ = A.take([128, 8, 1024], BF16)
        region2_end = A.off
        g1T, _ = A.take([128, 1024], F32)
        g2b, _ = A.take([128, 1024], F32)
        gmb, _ = A.take([128, 512], F32)
        cwm, _ = A.take([128, 32], F32)
        cbm, _ = A.take([128, 8], F32)
        bif, _ = A.take([128, 2], F32)
        ifr, _ = A.take([128, NT, 8], F32)
        tokS, _ = A.take([128, NT, 12], F32)
        Cf, _ = A.take([128, 4, 129], F32)
        Cb, _ = A.take([128, 4, 129], BF16)
        K2 = [A.take([128, 4, 128], BF16)[0] for _ in range(2)]
        spT = [A.take([128, 512], BF16)[0] for _ in range(2)]
        hmt, _ = A.take([128, 512], BF16)
        vmt = [A.take([128, 4, 129], BF16)[0] for _ in range(2)]
        ogt = [A.take([128, 512], F32)[0] for _ in range(2)]
        decb, _ = A.take([128, 4, 16], F32)
        sm, _ = A.take([128, 64], F32)
        rsm, _ = A.take([128, 64], F32)
        mixer_end = A.off
        rows = [A.take([128, 1024], F32, at=raw_at + i * 4096)[0] for i in range(4)]
        assert raw_at + 4 * 4096 <= rawdiag_end
        vst, _ = A.take([128, NT, 512], BF16, at=mixT_at + 4 * S * 2)
        vs, _ = A.take([128, NT, 512], BF16, at=raw_at)
        assert raw_at + NT * 512 * 2 <= rawdiag_end
        Ebuf = [A.take([128, 512], F32, at=wB_at + i * 2048)[0] for i in range(3)]
        ERbuf = [A.take([128, 512], F32, at=wB_at + 6144 + i * 2048)[0] for i in range(2)]
        SPbuf = [A.take([128, 512], BF16, at=wB_at + 10240 + i * 1024)[0] for i in range(3)]
        ATbuf = [A.take([128, 512], BF16, at=wB_at + 13312 + i * 1024)[0] for i in range(3)]

        RAWGRP = ["raw0", "raw1", "diagM"]
        ATTGRP = ["E", "ER", "SP", "AT"]

        for (ap, d_ap, nm) in [(g1T, g1T_d, "g1T"), (g2b, g2b_d, "g2b"), (gmb, gmb_d, "gmb"), (cwm, cwm_d, "cwm"),
                               (cbm, cbm_d, "cbm")]:
            P.op("sp", (lambda ap, d_ap: lambda e: e.dma_start(out=ap, in_=d_ap))(ap, d_ap), writes=[(nm,)], dma=True)
        P.op("sp", lambda e: e.dma_start(out=bif[0:4, :], in_=bif_d), writes=[("bif",)], dma=True)

        def load_w(dst, dst_tok, src, c0, ncols, eng="pool"):
            for kc in range(8):
                P.op(eng, (lambda kc: lambda e: e.dma_start(out=dst[:, kc, 0:ncols],
                                                            in_=src[kc * 128:(kc + 1) * 128, c0:c0 + ncols]))(kc),
                     writes=[(dst_tok, kc)], dma=True)

        fin = []
        for sq in range(nseq):
            P.alias(["wB"], ATTGRP)
            load_w(wA, "wA", w_in_d, 0, 1024)
            load_w(wB, "wB", w_in_d, 1024, 1032)
            if sq == 0:
                for tt in range(NT):
                    norm_transpose(x_d[sq, tt * 128:(tt + 1) * 128, :], [], tt % 2, g1T, ("g1T",), hT, ("hT", tt), tt * 128)

            P.alias(RAWGRP, ["vs", "rows"])
            P.alias(["vst"], ["mixT"])
            for cc in range(8):
                for j in range(4):
                    P.op("dve", (lambda cc, j: lambda e: e.tensor_scalar(out=diagM[:, cc, j, :], in0=ident_b,
                                                                         scalar1=cwm[:, cc * 4 + j:cc * 4 + j + 1],
                                                                         scalar2=None, op0=ALU.mult))(cc, j),
                         reads=CONST + [("cwm",)], writes=[("diagM",)])
            for rb, rawb in enumerate((raw0, raw1)):
                P.op("pool", (lambda rawb: lambda e: e.memset(rawb[:, 0:3], 0.0))(rawb), writes=[("raw%d" % rb, "halo")])
            for cc in range(8):
                rb = cc % 2
                rawb = (raw0, raw1)[rb]
                for tb in range(4):
                    bk = tb % 2
                    for kc in range(8):
                        P.op("pe", (lambda kc, cc, tb, bk: lambda e: e.matmul(
                            bank[bk], lhsT=wA[:, kc, cc * 128:(cc + 1) * 128], rhs=hT[:, kc, tb * 512:(tb + 1) * 512],
                            start=(kc == 0), stop=(kc == 7)))(kc, cc, tb, bk),
                            reads=[("wA", kc)] + [("hT", tb * 4 + i) for i in range(4)], writes=[PS(bk)])
                    P.op("act", (lambda rawb, tb, bk: lambda e: e.copy(out=rawb[:, 3 + tb * 512:3 + (tb + 1) * 512],
                                                                        in_=bank[bk]))(rawb, tb, bk),
                         reads=[PS(bk)], writes=[("raw%d" % rb, tb)])
                for tb in range(4):
                    bk = 2 + tb % 2
                    for j in range(4):
                        rd = [("raw%d" % rb, tb)] + ([("raw%d" % rb, tb - 1)] if tb > 0 else [("raw%d" % rb, "halo")])
                        if tb < 3:
                            rd.append(("raw%d" % rb, tb))
                        P.op("pe", (lambda cc, j, tb, bk, rawb: lambda e: e.matmul(
                            bank[bk], lhsT=diagM[:, cc, j, :], rhs=rawb[:, tb * 512 + j:tb * 512 + j + 512],
                            start=(j == 0), stop=(j == 3)))(cc, j, tb, bk, rawb),
                            reads=rd + [("diagM",)], writes=[PS(bk)])
                    P.op("act", (lambda cc, tb, bk: lambda e: e.activation(
                        out=qk[:, cc, tb * 512:(tb + 1) * 512], in_=bank[bk], func=AF.Silu,
                        bias=cbm[:, cc:cc + 1]))(cc, tb, bk),
                        reads=[PS(bk), ("cbm",)], writes=[("qk", cc, tb)])
            for tt in range(NT):
                for kc in range(8):
                    P.op("pe", (lambda kc, tt: lambda e: e.matmul(
                        bank[4][:, 0:8], lhsT=hT[:, kc, tt * 128:(tt + 1) * 128], rhs=wB[:, kc, 1024:1032],
                        start=(kc == 0), stop=(kc == 7)))(kc, tt),
                        reads=[("wB", kc), ("hT", tt)], writes=[PS(4)])
                P.op("dve", (lambda tt: lambda e: e.tensor_copy(out=ifr[:, tt, :], in_=bank[4][:, 0:8]))(tt),
                     reads=[PS(4)], writes=[("ifr", tt)])

            for tt in range(NT):
                bk = 2 + tt % 2
                for kc in range(8):
                    P.op("pe", (lambda kc, tt, bk: lambda e: e.matmul(
                        bank[bk], lhsT=hT[:, kc, tt * 128:(tt + 1) * 128], rhs=wB[:, kc, 0:512], start=(kc == 0),
                        stop=(kc == 7)))(kc, tt, bk),
                        reads=[("wB", kc), ("hT", tt)], writes=[PS(bk)])
                if tt % 2:
                    P.op("act", (lambda tt, bk: lambda e: e.copy(out=vst[:, tt, :], in_=bank[bk]))(tt, bk),
                         reads=[PS(bk)], writes=[("vst", tt)])
                else:
                    P.op("dve", (lambda tt, bk: lambda e: e.tensor_copy(out=vst[:, tt, :], in_=bank[bk]))(tt, bk),
                         reads=[PS(bk)], writes=[("vst", tt)])

            P.alias(["rows"], RAWGRP)
            lnk = -0.5 * math.log(128.0)
            R = lambda i: ("rows", i)
            P.op("dve", lambda e: e.memset(rsm[0:4, 0:8], 0.0), writes=[("rsm",)])
            for hh in range(2):
                for tl in range(8):
                    tt = hh * 8 + tl
                    for g in range(2):
                        P.op("pe", (lambda tt, tl, g: lambda e: e.transpose(
                            out=psum_t[0:4, g * 1024 + tl * 128:g * 1024 + (tl + 1) * 128],
                            in_=ifr[:, tt, g * 4:(g + 1) * 4], identity=ident_f))(tt, tl, g),
                            reads=[("ifr", tt)] + CONST, writes=[PS(2 * g + tl // 4)])
                P.op("dve", lambda e: e.tensor_scalar(out=rows[0][0:4], in0=psum_t[0:4, 0:1024], scalar1=bif[0:4, 0:1],
                                                      scalar2=None, op0=ALU.add),
                     reads=[PS(0), PS(1), ("bif",)], writes=[R(0)])
                P.op("dve", lambda e: e.tensor_scalar(out=rows[1][0:4], in0=psum_t[0:4, 1024:2048],
                                                      scalar1=bif[0:4, 1:2], scalar2=None, op0=ALU.add),
                     reads=[PS(2), PS(3), ("bif",)], writes=[R(1)])
                P.op("act", lambda e: e.activation(out=rows[1][0:4], in_=rows[1][0:4], func=AF.Exp, scale=-1.0),
                     reads=[R(1)], writes=[R(1)])
                P.op("act", lambda e: e.activation(out=rows[1][0:4], in_=rows[1][0:4], func=AF.Ln, bias=1.0),
                     reads=[R(1)], writes=[R(1)])
                P.op("dve", lambda e: e.tensor_scalar(out=rows[1][0:4], in0=rows[1][0:4], scalar1=-0.5, scalar2=None,
                                                      op0=ALU.mult),
                     reads=[R(1)], writes=[R(1)])
                P.op("dve", lambda e: e.tensor_tensor_scan(out=rows[2][0:4], data0=rows[1][0:4], data1=rows[1][0:4],
                                                           initial=rsm[0:4, 0:1], op0=ALU.add, op1=ALU.add),
                     reads=[R(1), ("rsm",)], writes=[R(2)])
                P.op("dve", lambda e: e.tensor_tensor(out=rows[0][0:4], in0=rows[0][0:4], in1=rows[2][0:4],
                                                      op=ALU.subtract),
                     reads=[R(0), R(2)], writes=[R(0)])
                P.op("dve", lambda e: e.tensor_tensor_scan(out=rows[1][0:4], data0=rows[0][0:4], data1=rows[0][0:4],
                                                           initial=rsm[0:4, 1:2], op0=ALU.max, op1=ALU.max),
                     reads=[R(0), R(1), ("rsm",)], writes=[R(1)])
                P.op("dve", lambda e: e.tensor_copy(out=rsm[0:4, 8:9], in_=rsm[0:4, 1:2]), reads=[("rsm",)],
                     writes=[("rsm",)])
                P.op("dve", lambda e: e.tensor_copy(out=rsm[0:4, 16:24], in_=rows[1][0:4, 127:1024:128]),
                     reads=[R(1), ("rsm",)], writes=[("rsm",)])
                P.op("dve", lambda e: e.tensor_copy(out=rsm[0:4, 9:16], in_=rsm[0:4, 16:23]), reads=[("rsm",)],
                     writes=[("rsm",)])
                P.op("dve", lambda e: e.tensor_copy(out=rsm[0:4, 0:1], in_=rows[2][0:4, 1023:1024]),
                     reads=[R(2), ("rsm",)], writes=[("rsm",)])
                P.op("dve", lambda e: e.tensor_copy(out=rsm[0:4, 1:2], in_=rows[1][0:4, 1023:1024]),
                     reads=[R(1), ("rsm",)], writes=[("rsm",)])
                P.op("dve", (lambda hh: lambda e: e.tensor_tensor(out=rsm[0:4, 24 + 8 * hh:32 + 8 * hh],
                                                                  in0=rsm[0:4, 8:16], in1=rsm[0:4, 16:24],
                                                                  op=ALU.subtract))(hh),
                     reads=[("rsm",)], writes=[("rsm",)])
                for q, (src, rcol, op, sc, bi) in enumerate([(0, 8, ALU.subtract, 1.0, lnk), (0, 16, ALU.subtract, 1.0, lnk),
                                                             (2, 8, ALU.add, -1.0, 0.0)]):
                    P.op("dve", (lambda src, rcol, op: lambda e: e.tensor_tensor(
                        out=rows[3][0:4].rearrange("p (c t) -> p c t", c=8),
                        in0=rows[src][0:4].rearrange("p (c t) -> p c t", c=8),
                        in1=rsm[0:4, rcol:rcol + 8].unsqueeze(2).broadcast_to([4, 8, 128]), op=op))(src, rcol, op),
                        reads=[R(src), ("rsm",)], writes=[R(3)])
                    P.op("act", (lambda sc, bi: lambda e: e.activation(out=rows[3][0:4], in_=rows[3][0:4], func=AF.Exp,
                                                                        scale=sc, bias=bi))(sc, bi),
                         reads=[R(3)], writes=[R(3)])
                    for tl in range(8):
                        P.op("pe", (lambda tl, q: lambda e: e.transpose(
                            out=bank[4][:, tl * 12 + q * 4:tl * 12 + q * 4 + 4], in_=rows[3][0:4, tl * 128:(tl + 1) * 128],
                            identity=ident_f[0:4, 0:4]))(tl, q),
                            reads=[R(3)] + CONST, writes=[PS(4)])
                P.op("dve", (lambda hh: lambda e: e.tensor_copy(
                    out=tokS[:, hh * 8:(hh + 1) * 8, :], in_=bank[4][:, 0:96].rearrange("p (a b) -> p a b", a=8)))(hh),
                    reads=[PS(4)], writes=[("tokS", hh)])
            P.op("act", lambda e: e.activation(out=rsm[0:4, 24:40], in_=rsm[0:4, 24:40], func=AF.Exp),
                 reads=[("rsm",)], writes=[("rsm",)])
            for h in range(4):
                P.op("pe", (lambda h: lambda e: e.matmul(bank[5][:, h * 16:(h + 1) * 16], lhsT=sel[0:4, h, :],
                                                         rhs=rsm[0:4, 24:40], start=True, stop=True))(h),
                     reads=[("rsm",)] + CONST, writes=[PS(5)])
            P.op("dve", lambda e: e.tensor_copy(out=decb, in_=bank[5][:, 0:64].rearrange("p (a b) -> p a b", a=4)),
                 reads=[PS(5)], writes=[("decb",)])
            P.op("dve", lambda e: e.memset(Cf, 0.0), writes=[("Cf",)])
            P.op("pool", lambda e: e.memset(Cb, 0.0), writes=[("Cb",)])
            for b2 in range(2):
                P.op("pool", (lambda b2: lambda e: e.memset(vmt[b2][:, :, 128:129], 1.0))(b2), writes=[("vmt", "ones", b2)])

            def W1a(c):
                tb = c // 4
                cs = slice(c * 128, (c + 1) * 128)
                b2 = c % 2
                P.op("act", lambda e: e.copy(out=vmt[b2][:, :, 0:128], in_=vst[:, c, :].rearrange("p (a b) -> p a b", a=4)),
                     reads=[("vst", c)], writes=[("vmt", b2)])
                for h in range(4):
                    P.op("pe", (lambda h: lambda e: e.matmul(bank[0][:, h * 128:(h + 1) * 128], lhsT=qk[:, 4 + h, cs],
                                                             rhs=qk[:, h, cs], start=True, stop=True))(h),
                         reads=[("qk", h, tb), ("qk", 4 + h, tb)], writes=[PS(0)])
                for h in range(4):
                    P.op("pe", (lambda h: lambda e: e.transpose(out=bankb[3][:, h * 128:(h + 1) * 128], in_=qk[:, 4 + h, cs],
                                                                identity=ident_b))(h),
                         reads=[("qk", 4 + h, tb)] + CONST, writes=[PS(3)])
                for h in range(4):
                    P.op("act", (lambda h: lambda e: e.activation(out=K2[b2][:, h, :], in_=bankb[3][:, h * 128:(h + 1) * 128],
                                                                  func=AF.Copy, scale=tokS[:, c, 4 + h:5 + h]))(h),
                         reads=[PS(3), ("tokS", c // 8)], writes=[("K2", b2, h)])
                for h in range(4):
                    P.op("dve", (lambda h: lambda e: e.scalar_tensor_tensor(
                        out=spT[b2][:, h * 128:(h + 1) * 128], in0=bank[0][:, h * 128:(h + 1) * 128],
                        scalar=tokS[:, c, h:h + 1], in1=maskM, op0=ALU.mult, op1=ALU.mult))(h),
                        reads=[PS(0), ("tokS", c // 8)] + CONST, writes=[("spT", b2, h)])
                for kc in range(8):
                    P.op("pe", (lambda kc: lambda e: e.matmul(bank[7], lhsT=hT[:, kc, cs], rhs=wB[:, kc, 512:1024],
                                                              start=(kc == 0), stop=(kc == 7)))(kc),
                         reads=[("wB", kc), ("hT", c)], writes=[PS(7)])
                og = ogt[b2]
                P.op("act", lambda e: e.activation(out=og, in_=bank[7], func=AF.Exp, scale=-1.0), reads=[PS(7)],
                     writes=[("ogt", b2)])
                P.op("act", lambda e: e.activation(out=og, in_=og, func=AF.Ln, bias=1.0), reads=[("ogt", b2)],
                     writes=[("ogt", b2)])
                P.op("act", lambda e: e.activation(out=og, in_=og, func=AF.Exp, scale=-1.0), reads=[("ogt", b2)],
                     writes=[("ogt", b2)])

            def W1c(c):
                b2 = c % 2
                og = ogt[b2]
                P.op("dve", lambda e: e.tensor_tensor(out=og, in0=og, in1=gmb, op=ALU.mult),
                     reads=[("ogt", b2), ("gmb",)], writes=[("ogt", b2)])

            def W1b(c):
                b2 = c % 2
                for h in range(4):
                    pkv = bank[4 + h // 2][:, (h % 2) * 129:(h % 2) * 129 + 129]
                    P.op("pe", (lambda h, pkv: lambda e: e.matmul(pkv, lhsT=K2[b2][:, h, :], rhs=vmt[b2][:, h, :], start=True,
                                                                  stop=True))(h, pkv),
                         reads=[("K2", b2, h), ("vmt", b2), ("vmt", "ones", b2)], writes=[PS(4 + h // 2)])

            def W2a(c):
                tb = c // 4
                cs = slice(c * 128, (c + 1) * 128)
                b2 = c % 2
                for h in range(4):
                    pn = bank[1 + h // 2][:, (h % 2) * 129:(h % 2) * 129 + 129]
                    P.op("pe", (lambda h, pn: lambda e: e.matmul(pn, lhsT=spT[b2][:, h * 128:(h + 1) * 128], rhs=vmt[b2][:, h, :],
                                                                 start=True, stop=False))(h, pn),
                         reads=[("spT", b2, h), ("vmt", b2), ("vmt", "ones", b2)], writes=[PS(1 + h // 2)])
                    P.op("pe", (lambda h, pn: lambda e: e.matmul(pn, lhsT=qk[:, h, cs], rhs=Cb[:, h, :], start=False,
                                                                 stop=True))(h, pn),
                         reads=[("qk", h, tb), ("Cb",)], writes=[PS(1 + h // 2)])
                for h in range(4):
                    pkv = bank[4 + h // 2][:, (h % 2) * 129:(h % 2) * 129 + 129]
                    P.op("dve", (lambda h, pkv: lambda e: e.scalar_tensor_tensor(
                        out=Cf[:, h, :], in0=Cf[:, h, :], scalar=decb[:, h, c:c + 1], in1=pkv, op0=ALU.mult,
                        op1=ALU.add))(h, pkv),
                        reads=[PS(4 + h // 2), ("decb",), ("Cf",)], writes=[("Cf",)])
                P.op("act", lambda e: e.copy(out=Cb, in_=Cf), reads=[("Cf",)], writes=[("Cb",)])

            def W2b(c):
                cs = slice(c * 128, (c + 1) * 128)
                b2 = c % 2
                og = ogt[b2]
                for hp in range(2):
                    den = bank[1 + hp][:, 0:258].rearrange("p (a b) -> p a b", a=2)[:, :, 128]
                    P.op("dve", (lambda hp, den: lambda e: e.tensor_scalar(
                        out=sm[:, 32 + 2 * hp:34 + 2 * hp], in0=den, scalar1=-1.0, scalar2=None, op0=ALU.mult))(hp, den),
                        reads=[PS(1 + hp)], writes=[("sm", "nd", hp)])
                    P.op("dve", (lambda hp, den: lambda e: e.tensor_tensor(
                        out=sm[:, 32 + 2 * hp:34 + 2 * hp], in0=den, in1=sm[:, 32 + 2 * hp:34 + 2 * hp], op=ALU.max))(hp, den),
                        reads=[PS(1 + hp), ("sm", "nd", hp)], writes=[("sm", "nd", hp)])
                P.op("dve", lambda e: e.tensor_tensor(out=sm[:, 0:4], in0=sm[:, 32:36], in1=tokS[:, c, 8:12], op=ALU.max),
                     reads=[("sm", "nd", 0), ("sm", "nd", 1), ("tokS", c // 8)], writes=[("sm", "m")])
                P.op("dve", lambda e: e.reciprocal(out=sm[:, 4:8], in_=sm[:, 0:4]), reads=[("sm", "m")], writes=[("sm", "r")])
                for h in range(4):
                    pn = bank[1 + h // 2][:, (h % 2) * 129:(h % 2) * 129 + 128]
                    P.op("act", (lambda h, pn: lambda e: e.activation(out=hmt[:, h * 128:(h + 1) * 128], in_=pn,
                                                                      func=AF.Square, accum_out=sm[:, 8 + h:9 + h]))(h, pn),
                         reads=[PS(1 + h // 2)], writes=[("hmt", h), ("sm", "ss", h)])
                P.op("dve", lambda e: e.tensor_tensor(out=sm[:, 12:16], in0=sm[:, 4:8], in1=sm[:, 4:8], op=ALU.mult),
                     reads=[("sm", "r")], writes=[("sm", "t")])
                P.op("dve", lambda e: e.tensor_tensor(out=sm[:, 12:16], in0=sm[:, 12:16], in1=sm[:, 8:12], op=ALU.mult),
                     reads=[("sm", "t")] + [("sm", "ss", h) for h in range(4)], writes=[("sm", "t")])
                P.op("act", lambda e: e.activation(out=sm[:, 16:20], in_=sm[:, 12:16], func=AF.Ln, scale=1.0 / 128, bias=EPS),
                     reads=[("sm", "t")], writes=[("sm", "ln")])
                P.op("act", lambda e: e.activation(out=sm[:, 20:24], in_=sm[:, 16:20], func=AF.Exp, scale=-0.5),
                     reads=[("sm", "ln")], writes=[("sm", "rs")])
                P.op("dve", lambda e: e.tensor_tensor(out=sm[:, 24:28], in0=sm[:, 20:24], in1=sm[:, 4:8], op=ALU.mult),
                     reads=[("sm", "rs"), ("sm", "r")], writes=[("sm", "fac")])
                for h in range(4):
                    pn = bank[1 + h // 2][:, (h % 2) * 129:(h % 2) * 129 + 128]
                    P.op("dve", (lambda h, pn: lambda e: e.scalar_tensor_tensor(
                        out=hmt[:, h * 128:(h + 1) * 128], in0=pn, scalar=sm[:, 24 + h:25 + h],
                        in1=og[:, h * 128:(h + 1) * 128], op0=ALU.mult, op1=ALU.mult))(h, pn),
                        reads=[PS(1 + h // 2), ("sm", "fac"), ("ogt", b2)], writes=[("hmt", h)])
                for h in range(4):
                    P.op("pe", (lambda h: lambda e: e.transpose(out=bankb[3][:, 512 + h * 128:512 + (h + 1) * 128],
                                                                in_=hmt[:, h * 128:(h + 1) * 128], identity=ident_b))(h),
                         reads=[("hmt", h)] + CONST, writes=[PS(3)])
                P.op("act", lambda e: e.copy(out=mixT[:, 0:4, cs],
                                             in_=bankb[3][:, 512:1024].rearrange("p (a b) -> p a b", a=4)),
                     reads=[PS(3)], writes=[("mixT", "m", c)])

            W1a(0)
            W1b(0)
            W1c(0)
            for c in range(NT):
                if c + 1 < NT:
                    W1a(c + 1)
                W2a(c)
                if c + 1 < NT:
                    W1b(c + 1)
                W2b(c)
                if c + 1 < NT:
                    W1c(c + 1)

            load_w(wA, "wA", w_in_d, 2056, 1024)
            load_w(wB, "wB", w_in_d, 3080, 512)
            P.alias(["vs"], RAWGRP + ["rows"])
            P.alias(["mixT"], ["vst"])
            for cc in range(8):
                for tb in range(4):
                    bk = tb % 2
                    for kc in range(8):
                        P.op("pe", (lambda kc, cc, tb, bk: lambda e: e.matmul(
                            bank[bk], lhsT=wA[:, kc, cc * 128:(cc + 1) * 128], rhs=hT[:, kc, tb * 512:(tb + 1) * 512],
                            start=(kc == 0), stop=(kc == 7)))(kc, cc, tb, bk),
                            reads=[("wA", kc)] + [("hT", tb * 4 + i) for i in range(4)], writes=[PS(bk)])
                    eng = "act" if tb % 2 == 0 else "dve"
                    if eng == "act":
                        P.op("act", (lambda cc, tb, bk: lambda e: e.copy(out=qk[:, cc, tb * 512:(tb + 1) * 512],
                                                                         in_=bank[bk]))(cc, tb, bk),
                             reads=[PS(bk)], writes=[("qk", cc, tb)])
                    else:
                        P.op("dve", (lambda cc, tb, bk: lambda e: e.tensor_copy(out=qk[:, cc, tb * 512:(tb + 1) * 512],
                                                                                in_=bank[bk]))(cc, tb, bk),
                             reads=[PS(bk)], writes=[("qk", cc, tb)])
            for tt in range(NT):
                bk = 2 + tt % 2
                for kc in range(8):
                    P.op("pe", (lambda kc, tt, bk: lambda e: e.matmul(
                        bank[bk], lhsT=hT[:, kc, tt * 128:(tt + 1) * 128], rhs=wB[:, kc, 0:512], start=(kc == 0),
                        stop=(kc == 7)))(kc, tt, bk),
                        reads=[("wB", kc), ("hT", tt)], writes=[PS(bk)])
                P.op("act" if tt % 2 else "dve",
                     (lambda tt, bk: (lambda e: e.copy(out=vs[:, tt, :], in_=bank[bk])) if tt % 2 else
                      (lambda e: e.tensor_copy(out=vs[:, tt, :], in_=bank[bk])))(tt, bk),
                     reads=[PS(bk)], writes=[("vs", tt)])

            load_w(wA, "wA", w_out_d, 0, 1024)
            P.alias(ATTGRP, ["wB"])
            units = []
            for h in range(8):
                for qt in range(4):
                    nblk = 4 * qt + 4
                    for bi, sb in enumerate(range(nblk - 1, -1, -1)):
                        i = sb - 4 * qt
                        units.append(dict(h=h, qt=qt, sb=sb, bi=bi, first=(bi == 0), last=(sb == 0), diag=(i >= 0),
                                          c0=(128 * i if i > 0 else 0)))
            NU = len(units)
            for k, u in enumerate(units):
                u["zb"] = k % 3
                u["e3"] = k % 3
                u["e2"] = k % 2
                u["pr"] = (u["h"] % 2) * 64
                u["qc"] = u["h"] // 2
                u["kc"] = 4 + u["h"] // 2
                u["pb"] = 3 + u["h"] % 2
                u["ob"] = 5 + u["qt"] % 2
                u["cols"] = slice(u["c0"], 512)

            def st_Z(u):
                zb, sb, cols, pr, qc, kc_, t0, c0 = u["zb"], u["sb"], u["cols"], u["pr"], u["qc"], u["kc"], u["qt"] * 512, u["c0"]
                diag = u["diag"]
                P.op("pe", lambda e: e.matmul(bank[zb][:, cols], lhsT=qk[pr:pr + 64, kc_, sb * 128:(sb + 1) * 128],
                                              rhs=qk[pr:pr + 64, qc, t0 + c0:t0 + 512], start=True, stop=not diag,
                                              skip_group_check=True),
                     reads=[("qk", qc, u["qt"]), ("qk", kc_, sb // 4)], writes=[PS(zb)])
                if diag:
                    P.op("pe", lambda e: e.matmul(bank[zb][:, c0:c0 + 128], lhsT=ident_b, rhs=negm, start=False, stop=True,
                                                  skip_group_check=True),
                         reads=CONST, writes=[PS(zb)])

            def st_E_SP(u):
                zb, cols, e3 = u["zb"], u["cols"], u["e3"]
                E, SP = Ebuf[e3], SPbuf[e3]
                P.op("act", lambda e: e.activation(out=E[:, cols], in_=bank[zb][:, cols], func=AF.Exp, scale=0.125),
                     reads=[PS(zb)], writes=[("E", e3)])
                P.op("act", lambda e: e.activation(out=SP[:, cols], in_=E[:, cols], func=AF.Ln, bias=1.0),
                     reads=[("E", e3)], writes=[("SP", e3)])

            def st_mm1(u):
                pb, cols, e3, first = u["pb"], u["cols"], u["e3"], u["first"]
                SP = SPbuf[e3]
                P.op("pe", lambda e: e.matmul(bank[pb][:, cols], lhsT=triU, rhs=SP[:, cols], start=first, stop=False,
                                              skip_group_check=True),
                     reads=[("SP", e3)] + CONST, writes=[PS(pb)])

            def st_mm2(u):
                if u["last"]:
                    return
                pb, cols, e3 = u["pb"], u["cols"], u["e3"]
                SP = SPbuf[e3]
                P.op("pe", lambda e: e.matmul(bank[pb][:, cols], lhsT=comp, rhs=SP[:, cols], start=False, stop=False,
                                              skip_group_check=True),
                     reads=[("SP", e3)] + CONST, writes=[PS(pb)])

            def st_ER(u):
                pb, cols, e2 = u["pb"], u["cols"], u["e2"]
                ER = ERbuf[e2]
                P.op("act", lambda e: e.activation(out=ER[:, cols], in_=bank[pb][:, cols], func=AF.Exp, scale=-1.0),
                     reads=[PS(pb)], writes=[("ER", e2)])

            def st_AT(u):
                cols, e3, e2 = u["cols"], u["e3"], u["e2"]
                E, ER, AT = Ebuf[e3], ERbuf[e2], ATbuf[e3]
                P.op("dve", lambda e: e.tensor_tensor(out=AT[:, cols], in0=E[:, cols], in1=ER[:, cols], op=ALU.mult),
                     reads=[("E", e3), ("ER", e2)], writes=[("AT", e3)])

            def st_AV(u):
                cols, e3, sb, h, pr, ob, first, last = u["cols"], u["e3"], u["sb"], u["h"], u["pr"], u["ob"], u["first"], u["last"]
                AT = ATbuf[e3]
                hp0 = (h // 2) * 128
                P.op("pe", lambda e: e.matmul(bank[ob][:, cols], lhsT=vs[:, sb, hp0:hp0 + 128], rhs=AT[:, cols],
                                              start=first, stop=last, skip_group_check=True),
                     reads=[("AT", e3), ("vs", sb)], writes=[PS(ob)])
                for _d in range(NDUMMY):
                    P.op("pe", lambda e: e.matmul(bank[7], lhsT=triU, rhs=ATbuf[e3][:, 0:512], start=True, stop=True,
                                                  skip_group_check=True),
                         reads=CONST, writes=[PS(7)])
                if last:
                    qc, t0, qt = u["qc"], u["qt"] * 512, u["qt"]
                    P.op("dve", lambda e: e.tensor_copy(out=mixT[pr:pr + 64, 4 + qc, t0:t0 + 512], in_=bank[ob][pr:pr + 64, :]),
                         reads=[PS(ob)], writes=[("mixT", "s", h, qt)])

            st_Z(units[0])
            st_Z(units[1])
            st_E_SP(units[0])
            for k in range(NU):
                if k >= 1:
                    st_mm2(units[k - 1])
                st_mm1(units[k])
                if k + 2 < NU:
                    st_Z(units[k + 2])
                if k >= 1:
                    st_AV(units[k - 1])
                if k + 1 < NU:
                    st_E_SP(units[k + 1])
                st_ER(units[k])
                st_AT(units[k])
                if sq + 1 < nseq and k % 20 == 10:
                    tt = k // 20
                    norm_transpose(x_d[sq + 1, tt * 128:(tt + 1) * 128, :], [], tt % 2, g1T, ("g1T",), hT, ("hT", tt), tt * 128)
            st_AV(units[NU - 1])

            if dbg_d is not None:
                P.op("sp", (lambda sq: lambda e: e.dma_start(out=dbg_d[sq], in_=mixT.rearrange("p a b -> p (a b)")))(sq),
                     reads=[("mixT", "m", c) for c in range(NT)] + [("mixT", "s", h, qt) for h in range(8) for qt in range(4)],
                     dma=True)

            for tt in range(NT):
                b = tt % 2
                yb = 2 * b
                xb4 = tt % 4
                P.op("sp", (lambda sq, tt, xb4: lambda e: e.dma_start(out=xt[xb4], in_=x_d[sq, tt * 128:(tt + 1) * 128, :]))(sq, tt, xb4),
                     writes=[("xt", xb4)], dma=True)
                mt = [("mixT", "m", tt)] + [("mixT", "s", h, tt // 4) for h in range(8)]
                for half in range(2):
                    for kc in range(8):
                        P.op("pe", (lambda kc, tt, half, yb: lambda e: e.matmul(
                            bank[yb + half], lhsT=mixT[:, kc, tt * 128:(tt + 1) * 128],
                            rhs=wA[:, kc, half * 512:(half + 1) * 512], start=(kc == 0), stop=(kc == 7)))(kc, tt, half, yb),
                            reads=[("wA", kc)] + mt, writes=[PS(yb + half)])
                me = post_norm_residual(yb, b, g2b, ("g2b",), [], out_d[sq, tt * 128:(tt + 1) * 128, :], [("out", sq, tt)],
                                        xb=xb4)
                if not do_ffn:
                    fin.append(me)

        if do_ffn:
            R1NAMES = ["hT", "qk", "raw0", "raw1", "diagM", "vs", "rows", "wB", "E", "ER", "SP", "AT"]
            R2NAMES = ["mixT", "vst", "wA"]
            R3NAMES = ["g1T", "g2b", "gmb", "cwm", "cbm", "bif", "ifr", "tokS", "Cf", "Cb", "K2", "spT", "hmt", "vmt", "ogt",
                       "decb", "sm", "rsm"]
            A.off = region1_at
            wup, _ = A.take([128, 8, 2 * DFF], BF16)
            h2T, _ = A.take([128, 8, FB], BF16)
            assert A.off <= region1_end, (A.off, region1_end)
            A.off = region1_end
            wdn, _ = A.take([128, NFF, D], BF16)
            g3T, _ = A.take([128, 1024], F32)
            assert A.off <= region2_end, (A.off, region2_end)
            A.off = region2_end
            g4b, _ = A.take([128, 1024], F32)
            cwf, _ = A.take([128, NFF * 3], F32)
            cbf, _ = A.take([128, NFF], F32)
            actT, _ = A.take([128, NFF, FB], BF16)
            graw = [A.take([128, 2 + FB], F32)[0] for _ in range(2)]
            cv = [A.take([128, FB], F32)[0] for _ in range(2)]
            gel = [A.take([128, FB], BF16)[0] for _ in range(2)]
            halo, _ = A.take([128, NFF, 2], F32)
            P.alias(["wupk0", "wupk1"], ["hT"])
            P.alias(["wupk%d" % k for k in range(2, 8)] + ["h2T"], R1NAMES)
            P.alias(["wdn", "g3T"], R2NAMES)
            P.alias(["g4b", "cwf", "cbf", "actT", "graw", "cv", "gel", "halo"], R3NAMES)
            for (ap, d_ap, nm) in [(g3T, g3T_d, "g3T"), (g4b, g4b_d, "g4b"), (cwf, cwf_d, "cwf"), (cbf, cbf_d, "cbf")]:
                P.op("sp", (lambda ap, d_ap: lambda e: e.dma_start(out=ap, in_=d_ap))(ap, d_ap), writes=[(nm,)], dma=True)
            HC = (NFF // 2) * 128
            assert 2 * (2 * DFF) * 2 <= 8 * S * 2
            for kcs in ((0, 1), (2, 3, 4, 5, 6, 7)):
                for sp_ in range(2):
                    for (nm, base) in (("g", 0), ("u", DFF)):
                        c0 = base + sp_ * HC
                        for kc in kcs:
                            P.op("pool", (lambda c0, kc: lambda e: e.dma_start(
                                out=wup[:, kc, c0:c0 + HC], in_=w_up_d[kc * 128:(kc + 1) * 128, c0:c0 + HC]))(c0, kc),
                                writes=[("wupk%d" % kc, nm, sp_)], dma=True)
            for j in range(NFF):
                P.op("pool", (lambda j: lambda e: e.dma_start(out=wdn[:, j, :], in_=w_dn_d[j * 128:(j + 1) * 128, :]))(j),
                     writes=[("wdn", j)], dma=True)
            nblk_seq = S // FB
            blocks = [(sq, blk) for sq in range(nseq) for blk in range(nblk_seq)]
            NB = len(blocks)

            TPB = FB // 128

            def f_norm_a(i, tl):
                sq, blk = blocks[i]
                tt = blk * TPB + tl
                xb = tl % 2
                P.op("sp", lambda e: e.dma_start(out=xt[xb], in_=out_d[sq, tt * 128:(tt + 1) * 128, :]),
                     reads=[("out", sq, tt)], writes=[("xt", xb)], dma=True)
                rstd, rtok = rms_rstd(xt[xb], [("xt", xb)], D, 4 * xb, xh[xb], ("xh", xb))
                P.op("dve", lambda e: e.tensor_scalar(out=xh[xb], in0=xt[xb], scalar1=rstd, scalar2=None, op0=ALU.mult),
                     reads=[("xt", xb), rtok], writes=[("xh", xb)])

            def f_norm_b(i, tl):
                xb = tl % 2
                for c in range(8):
                    P.op("pe", (lambda c: lambda e: e.transpose(out=bankb[0][:, c * 128:(c + 1) * 128],
                                                                in_=xh[xb][:, c * 128:(c + 1) * 128], identity=ident_b))(c),
                         reads=[("xh", xb)] + CONST, writes=[PS(0)])
                P.op("dve", lambda e: e.tensor_tensor(
                    out=h2T[:, :, tl * 128:(tl + 1) * 128], in0=bankb[0].rearrange("p (c t) -> p c t", c=8),
                    in1=g3T.rearrange("p (c t) -> p c t", c=8), op=ALU.mult),
                    reads=[PS(0), ("g3T",)], writes=[("h2T", tl)])

            def f_up(i, mid_hook):
                sq, blk = blocks[i]
                H2 = [("h2T", tl) for tl in range(TPB)]
                if blk == 0:
                    P.op("pool", lambda e: e.memset(halo, 0.0), writes=[("halo", j) for j in range(NFF)])
                for j in range(NFF):
                    gb = j % 2
                    for kc in range(8):
                        P.op("pe", (lambda kc, j, gb: lambda e: e.matmul(
                            bank[gb][:, 0:FB], lhsT=wup[:, kc, j * 128:(j + 1) * 128], rhs=h2T[:, kc, :],
                            start=(kc == 0), stop=(kc == 7)))(kc, j, gb),
                            reads=[("wupk%d" % kc, "g", j // (NFF // 2))] + H2, writes=[PS(gb)])
                    for kc in range(8):
                        P.op("pe", (lambda kc, j, gb: lambda e: e.matmul(
                            bank[2 + gb][:, 0:FB], lhsT=wup[:, kc, DFF + j * 128:DFF + (j + 1) * 128], rhs=h2T[:, kc, :],
                            start=(kc == 0), stop=(kc == 7)))(kc, j, gb),
                            reads=[("wupk%d" % kc, "u", j // (NFF // 2))] + H2, writes=[PS(2 + gb)])
                    P.op("pool", (lambda j, gb: lambda e: e.tensor_copy(out=graw[gb][:, 0:2], in_=halo[:, j, :]))(j, gb),
                         reads=[("halo", j)], writes=[("graw", gb, "h")])
                    P.op("act", (lambda gb: lambda e: e.copy(out=graw[gb][:, 2:2 + FB], in_=bank[gb][:, 0:FB]))(gb),
                         reads=[PS(gb)], writes=[("graw", gb)])
                    P.op("pool", (lambda j, gb: lambda e: e.tensor_copy(out=halo[:, j, :], in_=graw[gb][:, FB:FB + 2]))(j, gb),
                         reads=[("graw", gb)], writes=[("halo", j)])
                    GR = [("graw", gb), ("graw", gb, "h")]
                    P.op("dve", (lambda j, gb: lambda e: e.tensor_scalar(out=cv[gb], in0=graw[gb][:, 2:2 + FB],
                                                                         scalar1=cwf[:, j * 3 + 2:j * 3 + 3], scalar2=None,
                                                                         op0=ALU.mult))(j, gb),
                         reads=GR + [("cwf",)], writes=[("cv", gb)])
                    for tp in (1, 0):
                        P.op("dve", (lambda j, gb, tp: lambda e: e.scalar_tensor_tensor(
                            out=cv[gb], in0=graw[gb][:, tp:tp + FB], scalar=cwf[:, j * 3 + tp:j * 3 + tp + 1], in1=cv[gb],
                            op0=ALU.mult, op1=ALU.add))(j, gb, tp),
                            reads=GR + [("cwf",), ("cv", gb)], writes=[("cv", gb)])
                    P.op("act", (lambda j, gb: lambda e: e.activation(out=gel[gb], in_=cv[gb], func=AF.Gelu_apprx_tanh,
                                                                      bias=cbf[:, j:j + 1]))(j, gb),
                         reads=[("cv", gb), ("cbf",)], writes=[("gel", gb)])
                    P.op("dve", (lambda j, gb: lambda e: e.tensor_tensor(out=actT[:, j, :], in0=bank[2 + gb][:, 0:FB],
                                                                         in1=gel[gb], op=ALU.mult))(j, gb),
                         reads=[PS(2 + gb), ("gel", gb)], writes=[("actT", j)])
                    if j == 9:
                        mid_hook()

            def f_down(i, tl):
                sq, blk = blocks[i]
                AT_ALL = [("actT", j) for j in range(NFF)]
                tt = blk * TPB + tl
                yb = 4 + 2 * (tl % 2)
                xb = 2 + tl % 2
                P.op("sp", lambda e: e.dma_start(out=xt[xb], in_=out_d[sq, tt * 128:(tt + 1) * 128, :]),
                     reads=[("out", sq, tt)], writes=[("xt", xb)], dma=True)
                for half in range(2):
                    for j in range(NFF):
                        P.op("pe", (lambda j, half: lambda e: e.matmul(
                            bank[yb + half], lhsT=actT[:, j, tl * 128:(tl + 1) * 128],
                            rhs=wdn[:, j, half * 512:(half + 1) * 512], start=(j == 0), stop=(j == NFF - 1)))(j, half),
                            reads=[("wdn", j)] + AT_ALL, writes=[PS(yb + half)])
                me = post_norm_residual(yb, tl % 2, g4b, ("g4b",), [], out_d[sq, tt * 128:(tt + 1) * 128, :],
                                        [("out", sq, tt)], xb=xb, junk=(cv[tl % 2].bitcast(BF16), ("cv", tl % 2)))
                fin.append(me)

            for tl in range(TPB):
                f_norm_a(0, tl)
                f_norm_b(0, tl)
            for i in range(NB):
                nxt = i + 1 < NB

                def mid(i=i, nxt=nxt):
                    if nxt:
                        f_norm_a(i + 1, 0)
                        f_norm_a(i + 1, 1)
                f_up(i, mid)
                for tl in range(TPB):
                    f_down(i, tl)
                    if nxt:
                        f_norm_b(i + 1, tl)
                        if tl + 2 < TPB:
                            f_norm_a(i + 1, tl + 2)
        print("SBUF arena high-water (KB):", A.hi / 1024.0, " ops:", {e: len(P.ops[e]) for e in P.ENGS})
        P.emit(final_waits=fin)
    return nc


_CACHE = {}


def _prep_small(inputs):
    f = lambda a: np.ascontiguousarray(np.asarray(a, dtype=np.float32))
    gT = lambda g: f(np.broadcast_to(g.reshape(8, 128).T[:, :, None], (128, 8, 128)).reshape(128, 1024))
    gb = lambda g: f(np.broadcast_to(g.reshape(1, -1), (128, g.size)))
    d = {}
    d["g1T"] = gT(np.asarray(inputs["pre_mix_norm"])[0])
    d["g3T"] = gT(np.asarray(inputs["pre_ffn_norm"])[0])
    d["g2b"] = gb(np.asarray(inputs["post_mix_norm"])[0])
    d["g4b"] = gb(np.asarray(inputs["post_ffn_norm"])[0])
    d["gmb"] = gb(np.asarray(inputs["mlstm_norm"])[0])
    cw = np.asarray(inputs["mlstm_conv_w"])[0]
    d["cwm"] = f(cw.reshape(4, 8, 128).transpose(2, 1, 0).reshape(128, 32))
    d["cbm"] = f(np.asarray(inputs["mlstm_conv_b"])[0].reshape(8, 128).T)
    d["bif"] = f(np.stack([np.asarray(inputs["mlstm_b_i"])[0], np.asarray(inputs["mlstm_b_f"])[0]], axis=1))
    fw = np.asarray(inputs["ffn_conv_w"])[0]
    d["cwf"] = f(fw.reshape(3, NFF, 128).transpose(2, 1, 0).reshape(128, NFF * 3))
    d["cbf"] = f(np.asarray(inputs["ffn_conv_b"])[0].reshape(NFF, 128).T)
    return d


def kernel(**inputs):
    x = np.asarray(inputs["x"], dtype=np.float32)
    nseq = x.shape[0] // NCORES
    if "nc" not in _CACHE:
        _CACHE["nc"] = build_program(nseq=nseq)
    nc = _CACHE["nc"]
    small = _prep_small(inputs)
    shared = {
        "w_in": np.ascontiguousarray(np.asarray(inputs["w_in"], dtype=np.float32)[0]),
        "w_out": np.ascontiguousarray(np.asarray(inputs["w_out"], dtype=np.float32)[0]),
        "w_up": np.ascontiguousarray(np.asarray(inputs["w_up"], dtype=np.float32)[0]),
        "w_down": np.ascontiguousarray(np.asarray(inputs["w_down"], dtype=np.float32)[0]),
    }
    shared.update(small)
    in_maps = []
    for c in range(NCORES):
        m = dict(shared)
        m["x"] = np.ascontiguousarray(x[c * nseq:(c + 1) * nseq])
        in_maps.append(m)
    res = run_bass_kernel_spmd(nc, in_maps, core_ids=list(range(NCORES)))
    return np.concatenate([np.asarray(r["out"]) for r in res.results], axis=0).astype(np.float32)
```

```python
import contextlib
import math
import numpy as np
import concourse.bass as bass
import concourse.mybir as mybir
from concourse.bass_utils import run_bass_kernel_spmd

F32 = mybir.dt.float32
BF16 = mybir.dt.bfloat16
U8 = mybir.dt.uint8
AF = mybir.ActivationFunctionType
ALU = mybir.AluOpType

NCORES = 8
S = 2048
D = 1024
NT = S // 128
DFF = 2816
NFF = DFF // 128
INC = 3592
EPS = 1e-6
FB = 512
NDUMMY = 1


class Prog:
    ENGS = ("pe", "act", "dve", "pool", "sp")
    NDMA = 48

    def __init__(self, nc):
        self.nc = nc
        self.ops = {e: [] for e in self.ENGS}
        self.last_w = {}
        self.readers = {}
        self.ndma = 0
        self.alias_of = {}

    def alias(self, new_names, old_names):
        deps = set()
        old = set(old_names)
        for t, w in self.last_w.items():
            if t[0] in old and w is not None:
                deps.add(w)
        for t, rs in self.readers.items():
            if t[0] in old:
                deps.update(rs)
        deps = list(deps)
        new = set(new_names)
        for t in self.readers:
            if t[0] in new:
                self.readers[t] = self.readers[t] + deps
        for n in new_names:
            self.alias_of[n] = deps

    def _touch(self, t):
        if t not in self.last_w and t not in self.readers:
            self.last_w[t] = None
            self.readers[t] = list(self.alias_of.get(t[0], ()))

    def _dep(self, o, prod):
        if prod is None:
            return
        peng, pidx, pdma = prod
        if pdma is None and peng == o["eng"] and (peng == "pe" or pidx == o["idx"]):
            return
        o["waits"].add(prod)

    def op(self, eng, fn, reads=(), writes=(), dma=False):
        idx = len(self.ops[eng])
        o = {"eng": eng, "fn": fn, "waits": set(), "dma": None, "idx": idx, "inc": False}
        if dma:
            o["dma"] = self.ndma
            self.ndma += 1
        me = (eng, idx, o["dma"])
        writes = list(writes) + [t for t in reads if t[0] == "ps"]
        reads = [t for t in reads if t[0] != "ps"]
        for t in list(reads) + list(writes):
            self._touch(t)
        for t in reads:
            self._dep(o, self.last_w.get(t))
        for t in writes:
            self._dep(o, self.last_w.get(t))
            for r in self.readers.get(t, ()):
                if r != me:
                    self._dep(o, r)
        for t in reads:
            self.readers[t].append(me)
        for t in writes:
            self.last_w[t] = me
            self.readers[t] = []
        self.ops[eng].append(o)
        return me

    def emit(self, final_waits=()):
        nc = self.nc
        for e in self.ENGS:
            for o in self.ops[e]:
                for (pe, pi, pd) in o["waits"]:
                    if pd is None:
                        self.ops[pe][pi]["inc"] = True
        semval = {}
        for e in self.ENGS:
            c = 0
            for o in self.ops[e]:
                if o["dma"] is None and o["inc"]:
                    c += 1
                    semval[(e, o["idx"])] = c
        dma_slot, dma_val, dma_prev = {}, {}, {}
        slot_cnt = [0] * self.NDMA
        slot_last = [None] * self.NDMA
        order = [o for e in self.ENGS for o in self.ops[e] if o["dma"] is not None]
        order.sort(key=lambda o: o["dma"])
        half = self.NDMA // 2
        rr = {"hw": 0, "sw": 0}
        for o in order:
            if o["eng"] == "pool":
                s = half + rr["sw"] % half
                rr["sw"] += 1
            else:
                s = rr["hw"] % half
                rr["hw"] += 1
            slot_cnt[s] += 1
            dma_slot[o["dma"]] = s
            dma_val[o["dma"]] = 16 * slot_cnt[s]
            dma_prev[o["dma"]] = slot_last[s]
            slot_last[s] = o["dma"]

        with contextlib.ExitStack() as st:
            esem = {e: st.enter_context(nc.semaphore("s_" + e)) for e in ("pe", "act", "dve", "pool")}
            dsem = [st.enter_context(nc.semaphore("d%d" % i)) for i in range(self.NDMA)]
            block = st.enter_context(nc.Block())

            def run(ename, eng):
                seen = {}
                for o in self.ops[ename]:
                    waits = []
                    for (pe, pi, pd) in o["waits"]:
                        if pd is None:
                            waits.append((("e", pe), esem[pe], semval[(pe, pi)]))
                        else:
                            waits.append((("d", dma_slot[pd]), dsem[dma_slot[pd]], dma_val[pd]))
                    if o["dma"] is not None and dma_prev[o["dma"]] is not None:
                        pd = dma_prev[o["dma"]]
                        waits.append((("d", dma_slot[pd]), dsem[dma_slot[pd]], dma_val[pd]))
                    best = {}
                    for k, s, v in waits:
                        if seen.get(k, 0) >= v:
                            continue
                        if k not in best or best[k][1] < v:
                            best[k] = (s, v)
                    for k, (s, v) in best.items():
                        eng.wait_ge(s, v)
                        seen[k] = v
                    ins = o["fn"](eng)
                    if o["dma"] is not None:
                        ins.then_inc(dsem[dma_slot[o["dma"]]], 16)
                    elif o["inc"]:
                        ins.then_inc(esem[ename], 1)
                if ename == "sp":
                    for (pe, pi, pd) in final_waits:
                        eng.wait_ge(dsem[dma_slot[pd]], dma_val[pd])

            block.tensor(lambda eng: run("pe", eng))
            block.scalar(lambda eng: run("act", eng))
            block.vector(lambda eng: run("dve", eng))
            block.gpsimd(lambda eng: run("pool", eng))
            block.sync(lambda eng: run("sp", eng))


class Arena:
    def __init__(self, tensor, size):
        self.t = tensor
        self.size = size
        self.off = 0
        self.hi = 0

    def take(self, shape, dt, at=None):
        esz = {F32: 4, BF16: 2}[dt]
        n = int(np.prod(shape[1:])) * esz
        n_al = (n + 63) // 64 * 64
        if at is None:
            at = self.off
            self.off += n_al
        self.hi = max(self.hi, at + n_al)
        assert at + n_al <= self.size, ("SBUF arena overflow", at + n_al, self.size)
        v = self.t[:, at:at + n].bitcast(dt)
        if len(shape) == 3:
            v = v.rearrange("p (a b) -> p a b", a=shape[1])
        elif len(shape) == 4:
            v = v.rearrange("p (a b c) -> p a b c", a=shape[1], b=shape[2])
        return v, at


def build_program(nseq=2, do_ffn=True, debug_mix=False):
    nc = bass.Bass("TRN2", target_bir_lowering=False)
    dr = lambda name, shape, dt=F32, kind="ExternalInput": nc.dram_tensor(name, shape, dt, kind=kind).ap()
    x_d = dr("x", [nseq, S, D])
    w_in_d = dr("w_in", [D, INC])
    w_out_d = dr("w_out", [D, D])
    w_up_d = dr("w_up", [D, 2 * DFF])
    w_dn_d = dr("w_down", [DFF, D])
    g1T_d = dr("g1T", [128, 1024])
    g3T_d = dr("g3T", [128, 1024])
    g2b_d = dr("g2b", [128, 1024])
    g4b_d = dr("g4b", [128, 1024])
    gmb_d = dr("gmb", [128, 512])
    cwm_d = dr("cwm", [128, 32])
    cbm_d = dr("cbm", [128, 8])
    bif_d = dr("bif", [4, 2])
    cwf_d = dr("cwf", [128, NFF * 3])
    cbf_d = dr("cbf", [128, NFF])
    out_d = dr("out", [nseq, S, D], kind="ExternalOutput")
    dbg_d = dr("dbg", [nseq, 128, 8 * S], BF16, kind="ExternalOutput") if debug_mix else None

    P = Prog(nc)
    with contextlib.ExitStack() as st:
        ARENA = 206 * 1024
        arena_t = st.enter_context(nc.sbuf_tensor("arena", [128, ARENA], U8))
        A = Arena(arena_t, ARENA)
        psum_t = st.enter_context(nc.psum_tensor("psum", [128, 4096], F32))
        bank = [psum_t[:, i * 512:(i + 1) * 512] for i in range(8)]
        bankb = [b.bitcast(BF16) for b in bank]

        def PS(i):
            return ("ps", i)

        ident_b, _ = A.take([128, 128], BF16)
        ident_f, _ = A.take([128, 128], F32)
        maskM, _ = A.take([128, 128], BF16)
        maskS, _ = A.take([128, 128], F32)
        triU, _ = A.take([128, 128], BF16)
        comp, _ = A.take([128, 128], BF16)
        negm, _ = A.take([128, 128], BF16)
        sel, _ = A.take([128, 4, 128], F32)
        xt = [A.take([128, 1024], F32)[0] for _ in range(4)]
        xh = [A.take([128, 1024], BF16)[0] for _ in range(2)]
        stat, _ = A.take([128, 16], F32)
        persist_end = A.off

        def const_tri(ap, pattern, cm, op, base=0, val=1.0):
            P.op("pool", lambda e: e.memset(ap, val), writes=[("const",)])
            P.op("pool", lambda e: e.affine_select(out=ap, in_=ap, pattern=pattern, compare_op=op, fill=0.0,
                                                   base=base, channel_multiplier=cm),
                 reads=[("const",)], writes=[("const",)])

        const_tri(ident_b, [[-1, 128]], 1, ALU.is_equal)
        const_tri(ident_f, [[-1, 128]], 1, ALU.is_equal)
        const_tri(maskM, [[1, 128]], -1, ALU.is_ge)
        const_tri(maskS, [[1, 128]], -1, ALU.is_gt)
        const_tri(triU, [[-1, 128]], 1, ALU.is_ge)
        const_tri(comp, [[1, 128]], -1, ALU.is_gt)
        const_tri(sel[0:4], [[-1, 4], [0, 128]], 1, ALU.is_equal)
        const_tri(negm, [[-1, 128]], 1, ALU.is_ge, val=-2400.0)
        CONST = [("const",)]

        def rms_rstd(src_ap, src_tok, n, col, junk_ap, junk_tok):
            P.op("act", lambda e: e.activation(out=junk_ap, in_=src_ap, func=AF.Square,
                                               accum_out=stat[:, col:col + 1]),
                 reads=src_tok, writes=[junk_tok, ("stat", col)])
            P.op("act", lambda e: e.activation(out=stat[:, col + 1:col + 2], in_=stat[:, col:col + 1], func=AF.Ln,
                                               scale=1.0 / n, bias=EPS),
                 reads=[("stat", col)], writes=[("stat", col + 1)])
            P.op("act", lambda e: e.activation(out=stat[:, col + 2:col + 3], in_=stat[:, col + 1:col + 2],
                                               func=AF.Exp, scale=-0.5),
                 reads=[("stat", col + 1)], writes=[("stat", col + 2)])
            return stat[:, col + 2:col + 3], ("stat", col + 2)

        def norm_transpose(src_dram_tile, src_tok, b, gT, gT_tok, dstT, dst_tok, tcol):
            P.op("sp", lambda e: e.dma_start(out=xt[b], in_=src_dram_tile), reads=src_tok, writes=[("xt", b)], dma=True)
            rstd, rtok = rms_rstd(xt[b], [("xt", b)], D, 4 * b, xh[b], ("xh", b))
            P.op("dve", lambda e: e.tensor_scalar(out=xh[b], in0=xt[b], scalar1=rstd, scalar2=None, op0=ALU.mult),
                 reads=[("xt", b), rtok], writes=[("xh", b)])
            for c in range(8):
                P.op("pe", (lambda c: lambda e: e.transpose(out=bankb[7][:, c * 128:(c + 1) * 128],
                                                            in_=xh[b][:, c * 128:(c + 1) * 128], identity=ident_b))(c),
                     reads=[("xh", b)] + CONST, writes=[PS(7)])
            P.op("dve", lambda e: e.tensor_tensor(out=dstT[:, :, tcol:tcol + 128],
                                                  in0=bankb[7].rearrange("p (c t) -> p c t", c=8),
                                                  in1=gT.rearrange("p (c t) -> p c t", c=8), op=ALU.mult),
                 reads=[PS(7), gT_tok], writes=[dst_tok])

        def post_norm_residual(ybanks, b, gb, gb_tok, resid_tok_reads, out_dram_tile, out_tok, xb=None, junk=None):
            xb = b if xb is None else xb
            yap = psum_t[:, ybanks * 512:ybanks * 512 + 1024]
            ytok = [PS(ybanks), PS(ybanks + 1)]
            jap, jtok = junk if junk is not None else (xh[b], ("xh", b))
            rstd, rtok = rms_rstd(yap, ytok, D, 8 + 4 * b, jap, jtok)
            P.op("dve", lambda e: e.scalar_tensor_tensor(out=yap, in0=yap, scalar=rstd, in1=gb, op0=ALU.mult, op1=ALU.mult),
                 reads=ytok + [rtok, gb_tok], writes=[])
            P.op("dve", lambda e: e.tensor_tensor(out=xt[xb], in0=yap, in1=xt[xb], op=ALU.add),
                 reads=ytok + [("xt", xb)] + list(resid_tok_reads), writes=[("xt", xb)])
            return P.op("sp", lambda e: e.dma_start(out=out_dram_tile, in_=xt[xb]), reads=[("xt", xb)],
                        writes=out_tok, dma=True)

        persist_end = A.off

        A.off = persist_end
        region1_at = A.off
        hT, _ = A.take([128, 8, S], BF16)
        qk, _ = A.take([128, 8, S], BF16)
        raw0, raw_at = A.take([128, 3 + S], BF16)
        raw1, _ = A.take([128, 3 + S], BF16)
        diagM, _ = A.take([128, 8, 4, 128], BF16)
        rawdiag_end = A.off
        wB, wB_at = A.take([128, 8, 1032], BF16)
        region1_end = A.off
        mixT, mixT_at = A.take([128, 8, S], BF16)
        wA, _ = A.take([128, 8, 1024], BF16)
        region2_end = A.off
        g1T, _ = A.take([128, 1024], F32)
        g2b, _ = A.take([128, 1024], F32)
        gmb, _ = A.take([128, 512], F32)
        cwm, _ = A.take([128, 32], F32)
        cbm, _ = A.take([128, 8], F32)
        bif, _ = A.take([128, 2], F32)
        ifr, _ = A.take([128, NT, 8], F32)
        tokS, _ = A.take([128, NT, 12], F32)
        Cf, _ = A.take([128, 4, 129], F32)
        Cb, _ = A.take([128, 4, 129], BF16)
        K2 = [A.take([128, 4, 128], BF16)[0] for _ in range(2)]
        spT = [A.take([128, 512], BF16)[0] for _ in range(2)]
        hmt, _ = A.take([128, 512], BF16)
        vmt = [A.take([128, 4, 129], BF16)[0] for _ in range(2)]
        ogt = [A.take([128, 512], F32)[0] for _ in range(2)]
        decb, _ = A.take([128, 4, 16], F32)
        sm, _ = A.take([128, 64], F32)
        rsm, _ = A.take([128, 64], F32)
        mixer_end = A.off
        rows = [A.take([128, 1024], F32, at=raw_at + i * 4096)[0] for i in range(4)]
        assert raw_at + 4 * 4096 <= rawdiag_end
        vst, _ = A.take([128, NT, 512], BF16, at=mixT_at + 4 * S * 2)
        vs, _ = A.take([128, NT, 512], BF16, at=raw_at)
        assert raw_at + NT * 512 * 2 <= rawdiag_end
        Ebuf = [A.take([128, 512], F32, at=wB_at + i * 2048)[0] for i in range(3)]
        ERbuf = [A.take([128, 512], F32, at=wB_at + 6144 + i * 2048)[0] for i in range(2)]
        SPbuf = [A.take([128, 512], BF16, at=wB_at + 10240 + i * 1024)[0] for i in range(3)]
        ATbuf = [A.take([128, 512], BF16, at=wB_at + 13312 + i * 1024)[0] for i in range(3)]

        RAWGRP = ["raw0", "raw1", "diagM"]
        ATTGRP = ["E", "ER", "SP", "AT"]

        for (ap, d_ap, nm) in [(g1T, g1T_d, "g1T"), (g2b, g2b_d, "g2b"), (gmb, gmb_d, "gmb"), (cwm, cwm_d, "cwm"),
                               (cbm, cbm_d, "cbm")]:
            P.op("sp", (lambda ap, d_ap: lambda e: e.dma_start(out=ap, in_=d_ap))(ap, d_ap), writes=[(nm,)], dma=True)
        P.op("sp", lambda e: e.dma_start(out=bif[0:4, :], in_=bif_d), writes=[("bif",)], dma=True)

        def load_w(dst, dst_tok, src, c0, ncols, eng="pool"):
            for kc in range(8):
                P.op(eng, (lambda kc: lambda e: e.dma_start(out=dst[:, kc, 0:ncols],
                                                            in_=src[kc * 128:(kc + 1) * 128, c0:c0 + ncols]))(kc),
                     writes=[(dst_tok, kc)], dma=True)

        fin = []
        for sq in range(nseq):
            P.alias(["wB"], ATTGRP)
            load_w(wA, "wA", w_in_d, 0, 1024)
            load_w(wB, "wB", w_in_d, 1024, 1032)
            if sq == 0:
                for tt in range(NT):
                    norm_transpose(x_d[sq, tt * 128:(tt + 1) * 128, :], [], tt % 2, g1T, ("g1T",), hT, ("hT", tt), tt * 128)

            P.alias(RAWGRP, ["vs", "rows"])
            P.alias(["vst"], ["mixT"])
            for cc in range(8):
                for j in range(4):
                    P.op("dve", (lambda cc, j: lambda e: e.tensor_scalar(out=diagM[:, cc, j, :], in0=ident_b,
                                                                         scalar1=cwm[:, cc * 4 + j:cc * 4 + j + 1],
                                                                         scalar2=None, op0=ALU.mult))(cc, j),
                         reads=CONST + [("cwm",)], writes=[("diagM",)])
            for rb, rawb in enumerate((raw0, raw1)):
                P.op("pool", (lambda rawb: lambda e: e.memset(rawb[:, 0:3], 0.0))(rawb), writes=[("raw%d" % rb, "halo")])
            for cc in range(8):
                rb = cc % 2
                rawb = (raw0, raw1)[rb]
                for tb in range(4):
                    bk = tb % 2
                    for kc in range(8):
                        P.op("pe", (lambda kc, cc, tb, bk: lambda e: e.matmul(
                            bank[bk], lhsT=wA[:, kc, cc * 128:(cc + 1) * 128], rhs=hT[:, kc, tb * 512:(tb + 1) * 512],
                            start=(kc == 0), stop=(kc == 7)))(kc, cc, tb, bk),
                            reads=[("wA", kc)] + [("hT", tb * 4 + i) for i in range(4)], writes=[PS(bk)])
                    P.op("act", (lambda rawb, tb, bk: lambda e: e.copy(out=rawb[:, 3 + tb * 512:3 + (tb + 1) * 512],
                                                                        in_=bank[bk]))(rawb, tb, bk),
                         reads=[PS(bk)], writes=[("raw%d" % rb, tb)])
                for tb in range(4):
                    bk = 2 + tb % 2
                    for j in range(4):
                        rd = [("raw%d" % rb, tb)] + ([("raw%d" % rb, tb - 1)] if tb > 0 else [("raw%d" % rb, "halo")])
                        if tb < 3:
                            rd.append(("raw%d" % rb, tb))
                        P.op("pe", (lambda cc, j, tb, bk, rawb: lambda e: e.matmul(
                            bank[bk], lhsT=diagM[:, cc, j, :], rhs=rawb[:, tb * 512 + j:tb * 512 + j + 512],
                            start=(j == 0), stop=(j == 3)))(cc, j, tb, bk, rawb),
                            reads=rd + [("diagM",)], writes=[PS(bk)])
                    P.op("act", (lambda cc, tb, bk: lambda e: e.activation(
                        out=qk[:, cc, tb * 512:(tb + 1) * 512], in_=bank[bk], func=AF.Silu,
                        bias=cbm[:, cc:cc + 1]))(cc, tb, bk),
                        reads=[PS(bk), ("cbm",)], writes=[("qk", cc, tb)])
            for tt in range(NT):
                for kc in range(8):
                    P.op("pe", (lambda kc, tt: lambda e: e.matmul(
                        bank[4][:, 0:8], lhsT=hT[:, kc, tt * 128:(tt + 1) * 128], rhs=wB[:, kc, 1024:1032],
                        start=(kc == 0), stop=(kc == 7)))(kc, tt),
                        reads=[("wB", kc), ("hT", tt)], writes=[PS(4)])
                P.op("dve", (lambda tt: lambda e: e.tensor_copy(out=ifr[:, tt, :], in_=bank[4][:, 0:8]))(tt),
                     reads=[PS(4)], writes=[("ifr", tt)])

            for tt in range(NT):
                bk = 2 + tt % 2
                for kc in range(8):
                    P.op("pe", (lambda kc, tt, bk: lambda e: e.matmul(
                        bank[bk], lhsT=hT[:, kc, tt * 128:(tt + 1) * 128], rhs=wB[:, kc, 0:512], start=(kc == 0),
                        stop=(kc == 7)))(kc, tt, bk),
                        reads=[("wB", kc), ("hT", tt)], writes=[PS(bk)])
                if tt % 2:
                    P.op("act", (lambda tt, bk: lambda e: e.copy(out=vst[:, tt, :], in_=bank[bk]))(tt, bk),
                         reads=[PS(bk)], writes=[("vst", tt)])
                else:
                    P.op("dve", (lambda tt, bk: lambda e: e.tensor_copy(out=vst[:, tt, :], in_=bank[bk]))(tt, bk),
                         reads=[PS(bk)], writes=[("vst", tt)])

            P.alias(["rows"], RAWGRP)
            lnk = -0.5 * math.log(128.0)
            R = lambda i: ("rows", i)
            P.op("dve", lambda e: e.memset(rsm[0:4, 0:8], 0.0), writes=[("rsm",)])
            for hh in range(2):
                for tl in range(8):
                    tt = hh * 8 + tl
                    for g in range(2):
                        P.op("pe", (lambda tt, tl, g: lambda e: e.transpose(
                            out=psum_t[0:4, g * 1024 + tl * 128:g * 1024 + (tl + 1) * 128],
                            in_=ifr[:, tt, g * 4:(g + 1) * 4], identity=ident_f))(tt, tl, g),
                            reads=[("ifr", tt)] + CONST, writes=[PS(2 * g + tl // 4)])
                P.op("dve", lambda e: e.tensor_scalar(out=rows[0][0:4], in0=psum_t[0:4, 0:1024], scalar1=bif[0:4, 0:1],
                                                      scalar2=None, op0=ALU.add),
                     reads=[PS(0), PS(1), ("bif",)], writes=[R(0)])
                P.op("dve", lambda e: e.tensor_scalar(out=rows[1][0:4], in0=psum_t[0:4, 1024:2048],
                                                      scalar1=bif[0:4, 1:2], scalar2=None, op0=ALU.add),
                     reads=[PS(2), PS(3), ("bif",)], writes=[R(1)])
                P.op("act", lambda e: e.activation(out=rows[1][0:4], in_=rows[1][0:4], func=AF.Exp, scale=-1.0),
                     reads=[R(1)], writes=[R(1)])
                P.op("act", lambda e: e.activation(out=rows[1][0:4], in_=rows[1][0:4], func=AF.Ln, bias=1.0),
                     reads=[R(1)], writes=[R(1)])
                P.op("dve", lambda e: e.tensor_scalar(out=rows[1][0:4], in0=rows[1][0:4], scalar1=-0.5, scalar2=None,
                                                      op0=ALU.mult),
                     reads=[R(1)], writes=[R(1)])
                P.op("dve", lambda e: e.tensor_tensor_scan(out=rows[2][0:4], data0=rows[1][0:4], data1=rows[1][0:4],
                                                           initial=rsm[0:4, 0:1], op0=ALU.add, op1=ALU.add),
                     reads=[R(1), ("rsm",)], writes=[R(2)])
                P.op("dve", lambda e: e.tensor_tensor(out=rows[0][0:4], in0=rows[0][0:4], in1=rows[2][0:4],
                                                      op=ALU.subtract),
                     reads=[R(0), R(2)], writes=[R(0)])
                P.op("dve", lambda e: e.tensor_tensor_scan(out=rows[1][0:4], data0=rows[0][0:4], data1=rows[0][0:4],
                                                           initial=rsm[0:4, 1:2], op0=ALU.max, op1=ALU.max),
                     reads=[R(0), R(1), ("rsm",)], writes=[R(1)])
                P.op("dve", lambda e: e.tensor_copy(out=rsm[0:4, 8:9], in_=rsm[0:4, 1:2]), reads=[("rsm",)],
                     writes=[("rsm",)])
                P.op("dve", lambda e: e.tensor_copy(out=rsm[0:4, 16:24], in_=rows[1][0:4, 127:1024:128]),
                     reads=[R(1), ("rsm",)], writes=[("rsm",)])
                P.op("dve", lambda e: e.tensor_copy(out=rsm[0:4, 9:16], in_=rsm[0:4, 16:23]), reads=[("rsm",)],
                     writes=[("rsm",)])
                P.op("dve", lambda e: e.tensor_copy(out=rsm[0:4, 0:1], in_=rows[2][0:4, 1023:1024]),
                     reads=[R(2), ("rsm",)], writes=[("rsm",)])
                P.op("dve", lambda e: e.tensor_copy(out=rsm[0:4, 1:2], in_=rows[1][0:4, 1023:1024]),
                     reads=[R(1), ("rsm",)], writes=[("rsm",)])
                P.op("dve", (lambda hh: lambda e: e.tensor_tensor(out=rsm[0:4, 24 + 8 * hh:32 + 8 * hh],
                                                                  in0=rsm[0:4, 8:16], in1=rsm[0:4, 16:24],
                                                                  op=ALU.subtract))(hh),
                     reads=[("rsm",)], writes=[("rsm",)])
                for q, (src, rcol, op, sc, bi) in enumerate([(0, 8, ALU.subtract, 1.0, lnk), (0, 16, ALU.subtract, 1.0, lnk),
                                                             (2, 8, ALU.add, -1.0, 0.0)]):
                    P.op("dve", (lambda src, rcol, op: lambda e: e.tensor_tensor(
                        out=rows[3][0:4].rearrange("p (c t) -> p c t", c=8),
                        in0=rows[src][0:4].rearrange("p (c t) -> p c t", c=8),
                        in1=rsm[0:4, rcol:rcol + 8].unsqueeze(2).broadcast_to([4, 8, 128]), op=op))(src, rcol, op),
                        reads=[R(src), ("rsm",)], writes=[R(3)])
                    P.op("act", (lambda sc, bi: lambda e: e.activation(out=rows[3][0:4], in_=rows[3][0:4], func=AF.Exp,
                                                                        scale=sc, bias=bi))(sc, bi),
                         reads=[R(3)], writes=[R(3)])
                    for tl in range(8):
                        P.op("pe", (lambda tl, q: lambda e: e.transpose(
                            out=bank[4][:, tl * 12 + q * 4:tl * 12 + q * 4 + 4], in_=rows[3][0:4, tl * 128:(tl + 1) * 128],
                            identity=ident_f[0:4, 0:4]))(tl, q),
                            reads=[R(3)] + CONST, writes=[PS(4)])
                P.op("dve", (lambda hh: lambda e: e.tensor_copy(
                    out=tokS[:, hh * 8:(hh + 1) * 8, :], in_=bank[4][:, 0:96].rearrange("p (a b) -> p a b", a=8)))(hh),
                    reads=[PS(4)], writes=[("tokS", hh)])
            P.op("act", lambda e: e.activation(out=rsm[0:4, 24:40], in_=rsm[0:4, 24:40], func=AF.Exp),
                 reads=[("rsm",)], writes=[("rsm",)])
            for h in range(4):
                P.op("pe", (lambda h: lambda e: e.matmul(bank[5][:, h * 16:(h + 1) * 16], lhsT=sel[0:4, h, :],
                                                         rhs=rsm[0:4, 24:40], start=True, stop=True))(h),
                     reads=[("rsm",)] + CONST, writes=[PS(5)])
            P.op("dve", lambda e: e.tensor_copy(out=decb, in_=bank[5][:, 0:64].rearrange("p (a b) -> p a b", a=4)),
                 reads=[PS(5)], writes=[("decb",)])
            P.op("dve", lambda e: e.memset(Cf, 0.0), writes=[("Cf",)])
            P.op("pool", lambda e: e.memset(Cb, 0.0), writes=[("Cb",)])
            for b2 in range(2):
                P.op("pool", (lambda b2: lambda e: e.memset(vmt[b2][:, :, 128:129], 1.0))(b2), writes=[("vmt", "ones", b2)])

            def W1a(c):
                tb = c // 4
                cs = slice(c * 128, (c + 1) * 128)
                b2 = c % 2
                P.op("act", lambda e: e.copy(out=vmt[b2][:, :, 0:128], in_=vst[:, c, :].rearrange("p (a b) -> p a b", a=4)),
                     reads=[("vst", c)], writes=[("vmt", b2)])
                for h in range(4):
                    P.op("pe", (lambda h: lambda e: e.matmul(bank[0][:, h * 128:(h + 1) * 128], lhsT=qk[:, 4 + h, cs],
                                                             rhs=qk[:, h, cs], start=True, stop=True))(h),
                         reads=[("qk", h, tb), ("qk", 4 + h, tb)], writes=[PS(0)])
                for h in range(4):
                    P.op("pe", (lambda h: lambda e: e.transpose(out=bankb[3][:, h * 128:(h + 1) * 128], in_=qk[:, 4 + h, cs],
                                                                identity=ident_b))(h),
                         reads=[("qk", 4 + h, tb)] + CONST, writes=[PS(3)])
                for h in range(4):
                    P.op("act", (lambda h: lambda e: e.activation(out=K2[b2][:, h, :], in_=bankb[3][:, h * 128:(h + 1) * 128],
                                                                  func=AF.Copy, scale=tokS[:, c, 4 + h:5 + h]))(h),
                         reads=[PS(3), ("tokS", c // 8)], writes=[("K2", b2, h)])
                for h in range(4):
                    P.op("dve", (lambda h: lambda e: e.scalar_tensor_tensor(
                        out=spT[b2][:, h * 128:(h + 1) * 128], in0=bank[0][:, h * 128:(h + 1) * 128],
                        scalar=tokS[:, c, h:h + 1], in1=maskM, op0=ALU.mult, op1=ALU.mult))(h),
                        reads=[PS(0), ("tokS", c // 8)] + CONST, writes=[("spT", b2, h)])
                for kc in range(8):
                    P.op("pe", (lambda kc: lambda e: e.matmul(bank[7], lhsT=hT[:, kc, cs], rhs=wB[:, kc, 512:1024],
                                                              start=(kc == 0), stop=(kc == 7)))(kc),
                         reads=[("wB", kc), ("hT", c)], writes=[PS(7)])
                og = ogt[b2]
                P.op("act", lambda e: e.activation(out=og, in_=bank[7], func=AF.Exp, scale=-1.0), reads=[PS(7)],
                     writes=[("ogt", b2)])
                P.op("act", lambda e: e.activation(out=og, in_=og, func=AF.Ln, bias=1.0), reads=[("ogt", b2)],
                     writes=[("ogt", b2)])
                P.op("act", lambda e: e.activation(out=og, in_=og, func=AF.Exp, scale=-1.0), reads=[("ogt", b2)],
                     writes=[("ogt", b2)])

            def W1c(c):
                b2 = c % 2
                og = ogt[b2]
                P.op("dve", lambda e: e.tensor_tensor(out=og, in0=og, in1=gmb, op=ALU.mult),
                     reads=[("ogt", b2), ("gmb",)], writes=[("ogt", b2)])

            def W1b(c):
                b2 = c % 2
                for h in range(4):
                    pkv = bank[4 + h // 2][:, (h % 2) * 129:(h % 2) * 129 + 129]
                    P.op("pe", (lambda h, pkv: lambda e: e.matmul(pkv, lhsT=K2[b2][:, h, :], rhs=vmt[b2][:, h, :], start=True,
                                                                  stop=True))(h, pkv),
                         reads=[("K2", b2, h), ("vmt", b2), ("vmt", "ones", b2)], writes=[PS(4 + h // 2)])

            def W2a(c):
                tb = c // 4
                cs = slice(c * 128, (c + 1) * 128)
                b2 = c % 2
                for h in range(4):
                    pn = bank[1 + h // 2][:, (h % 2) * 129:(h % 2) * 129 + 129]
                    P.op("pe", (lambda h, pn: lambda e: e.matmul(pn, lhsT=spT[b2][:, h * 128:(h + 1) * 128], rhs=vmt[b2][:, h, :],
                                                                 start=True, stop=False))(h, pn),
                         reads=[("spT", b2, h), ("vmt", b2), ("vmt", "ones", b2)], writes=[PS(1 + h // 2)])
                    P.op("pe", (lambda h, pn: lambda e: e.matmul(pn, lhsT=qk[:, h, cs], rhs=Cb[:, h, :], start=False,
                                                                 stop=True))(h, pn),
                         reads=[("qk", h, tb), ("Cb",)], writes=[PS(1 + h // 2)])
                for h in range(4):
                    pkv = bank[4 + h // 2][:, (h % 2) * 129:(h % 2) * 129 + 129]
                    P.op("dve", (lambda h, pkv: lambda e: e.scalar_tensor_tensor(
                        out=Cf[:, h, :], in0=Cf[:, h, :], scalar=decb[:, h, c:c + 1], in1=pkv, op0=ALU.mult,
                        op1=ALU.add))(h, pkv),
                        reads=[PS(4 + h // 2), ("decb",), ("Cf",)], writes=[("Cf",)])
                P.op("act", lambda e: e.copy(out=Cb, in_=Cf), reads=[("Cf",)], writes=[("Cb",)])

            def W2b(c):
                cs = slice(c * 128, (c + 1) * 128)
                b2 = c % 2
                og = ogt[b2]
                for hp in range(2):
                    den = bank[1 + hp][:, 0:258].rearrange("p (a b) -> p a b", a=2)[:, :, 128]
                    P.op("dve", (lambda hp, den: lambda e: e.tensor_scalar(
                        out=sm[:, 32 + 2 * hp:34 + 2 * hp], in0=den, scalar1=-1.0, scalar2=None, op0=ALU.mult))(hp, den),
                        reads=[PS(1 + hp)], writes=[("sm", "nd", hp)])
                    P.op("dve", (lambda hp, den: lambda e: e.tensor_tensor(
                        out=sm[:, 32 + 2 * hp:34 + 2 * hp], in0=den, in1=sm[:, 32 + 2 * hp:34 + 2 * hp], op=ALU.max))(hp, den),
                        reads=[PS(1 + hp), ("sm", "nd", hp)], writes=[("sm", "nd", hp)])
                P.op("dve", lambda e: e.tensor_tensor(out=sm[:, 0:4], in0=sm[:, 32:36], in1=tokS[:, c, 8:12], op=ALU.max),
                     reads=[("sm", "nd", 0), ("sm", "nd", 1), ("tokS", c // 8)], writes=[("sm", "m")])
                P.op("dve", lambda e: e.reciprocal(out=sm[:, 4:8], in_=sm[:, 0:4]), reads=[("sm", "m")], writes=[("sm", "r")])
                for h in range(4):
                    pn = bank[1 + h // 2][:, (h % 2) * 129:(h % 2) * 129 + 128]
                    P.op("act", (lambda h, pn: lambda e: e.activation(out=hmt[:, h * 128:(h + 1) * 128], in_=pn,
                                                                      func=AF.Square, accum_out=sm[:, 8 + h:9 + h]))(h, pn),
                         reads=[PS(1 + h // 2)], writes=[("hmt", h), ("sm", "ss", h)])
                P.op("dve", lambda e: e.tensor_tensor(out=sm[:, 12:16], in0=sm[:, 4:8], in1=sm[:, 4:8], op=ALU.mult),
                     reads=[("sm", "r")], writes=[("sm", "t")])
                P.op("dve", lambda e: e.tensor_tensor(out=sm[:, 12:16], in0=sm[:, 12:16], in1=sm[:, 8:12], op=ALU.mult),
                     reads=[("sm", "t")] + [("sm", "ss", h) for h in range(4)], writes=[("sm", "t")])
                P.op("act", lambda e: e.activation(out=sm[:, 16:20], in_=sm[:, 12:16], func=AF.Ln, scale=1.0 / 128, bias=EPS),
                     reads=[("sm", "t")], writes=[("sm", "ln")])
                P.op("act", lambda e: e.activation(out=sm[:, 20:24], in_=sm[:, 16:20], func=AF.Exp, scale=-0.5),
                     reads=[("sm", "ln")], writes=[("sm", "rs")])
                P.op("dve", lambda e: e.tensor_tensor(out=sm[:, 24:28], in0=sm[:, 20:24], in1=sm[:, 4:8], op=ALU.mult),
                     reads=[("sm", "rs"), ("sm", "r")], writes=[("sm", "fac")])
                for h in range(4):
                    pn = bank[1 + h // 2][:, (h % 2) * 129:(h % 2) * 129 + 128]
                    P.op("dve", (lambda h, pn: lambda e: e.scalar_tensor_tensor(
                        out=hmt[:, h * 128:(h + 1) * 128], in0=pn, scalar=sm[:, 24 + h:25 + h],
                        in1=og[:, h * 128:(h + 1) * 128], op0=ALU.mult, op1=ALU.mult))(h, pn),
                        reads=[PS(1 + h // 2), ("sm", "fac"), ("ogt", b2)], writes=[("hmt", h)])
                for h in range(4):
                    P.op("pe", (lambda h: lambda e: e.transpose(out=bankb[3][:, 512 + h * 128:512 + (h + 1) * 128],
                                                                in_=hmt[:, h * 128:(h + 1) * 128], identity=ident_b))(h),
                         reads=[("hmt", h)] + CONST, writes=[PS(3)])
                P.op("act", lambda e: e.copy(out=mixT[:, 0:4, cs],
                                             in_=bankb[3][:, 512:1024].rearrange("p (a b) -> p a b", a=4)),
                     reads=[PS(3)], writes=[("mixT", "m", c)])

            W1a(0)
            W1b(0)
            W1c(0)
            for c in range(NT):
                if c + 1 < NT:
                    W1a(c + 1)
                W2a(c)
                if c + 1 < NT:
                    W1b(c + 1)
                W2b(c)
                if c + 1 < NT:
                    W1c(c + 1)

            load_w(wA, "wA", w_in_d, 2056, 1024)
            load_w(wB, "wB", w_in_d, 3080, 512)
            P.alias(["vs"], RAWGRP + ["rows"])
            P.alias(["mixT"], ["vst"])
            for cc in range(8):
                for tb in range(4):
                    bk = tb % 2
                    for kc in range(8):
                        P.op("pe", (lambda kc, cc, tb, bk: lambda e: e.matmul(
                            bank[bk], lhsT=wA[:, kc, cc * 128:(cc + 1) * 128], rhs=hT[:, kc, tb * 512:(tb + 1) * 512],
                            start=(kc == 0), stop=(kc == 7)))(kc, cc, tb, bk),
                            reads=[("wA", kc)] + [("hT", tb * 4 + i) for i in range(4)], writes=[PS(bk)])
                    eng = "act" if tb % 2 == 0 else "dve"
                    if eng == "act":
                        P.op("act", (lambda cc, tb, bk: lambda e: e.copy(out=qk[:, cc, tb * 512:(tb + 1) * 512],
                                                                         in_=bank[bk]))(cc, tb, bk),
                             reads=[PS(bk)], writes=[("qk", cc, tb)])
                    else:
                        P.op("dve", (lambda cc, tb, bk: lambda e: e.tensor_copy(out=qk[:, cc, tb * 512:(tb + 1) * 512],
                                                                                in_=bank[bk]))(cc, tb, bk),
                             reads=[PS(bk)], writes=[("qk", cc, tb)])
            for tt in range(NT):
                bk = 2 + tt % 2
                for kc in range(8):
                    P.op("pe", (lambda kc, tt, bk: lambda e: e.matmul(
                        bank[bk], lhsT=hT[:, kc, tt * 128:(tt + 1) * 128], rhs=wB[:, kc, 0:512], start=(kc == 0),
                        stop=(kc == 7)))(kc, tt, bk),
                        reads=[("wB", kc), ("hT", tt)], writes=[PS(bk)])
                P.op("act" if tt % 2 else "dve",
                     (lambda tt, bk: (lambda e: e.copy(out=vs[:, tt, :], in_=bank[bk])) if tt % 2 else
                      (lambda e: e.tensor_copy(out=vs[:, tt, :], in_=bank[bk])))(tt, bk),
                     reads=[PS(bk)], writes=[("vs", tt)])

            load_w(wA, "wA", w_out_d, 0, 1024)
            P.alias(ATTGRP, ["wB"])
            units = []
            for h in range(8):
                for qt in range(4):
                    nblk = 4 * qt + 4
                    for bi, sb in enumerate(range(nblk - 1, -1, -1)):
                        i = sb - 4 * qt
                        units.append(dict(h=h, qt=qt, sb=sb, bi=bi, first=(bi == 0), last=(sb == 0), diag=(i >= 0),
                                          c0=(128 * i if i > 0 else 0)))
            NU = len(units)
            for k, u in enumerate(units):
                u["zb"] = k % 3
                u["e3"] = k % 3
                u["e2"] = k % 2
                u["pr"] = (u["h"] % 2) * 64
                u["qc"] = u["h"] // 2
                u["kc"] = 4 + u["h"] // 2
                u["pb"] = 3 + u["h"] % 2
                u["ob"] = 5 + u["qt"] % 2
                u["cols"] = slice(u["c0"], 512)

            def st_Z(u):
                zb, sb, cols, pr, qc, kc_, t0, c0 = u["zb"], u["sb"], u["cols"], u["pr"], u["qc"], u["kc"], u["qt"] * 512, u["c0"]
                diag = u["diag"]
                P.op("pe", lambda e: e.matmul(bank[zb][:, cols], lhsT=qk[pr:pr + 64, kc_, sb * 128:(sb + 1) * 128],
                                              rhs=qk[pr:pr + 64, qc, t0 + c0:t0 + 512], start=True, stop=not diag,
                                              skip_group_check=True),
                     reads=[("qk", qc, u["qt"]), ("qk", kc_, sb // 4)], writes=[PS(zb)])
                if diag:
                    P.op("pe", lambda e: e.matmul(bank[zb][:, c0:c0 + 128], lhsT=ident_b, rhs=negm, start=False, stop=True,
                                                  skip_group_check=True),
                         reads=CONST, writes=[PS(zb)])

            def st_E_SP(u):
                zb, cols, e3 = u["zb"], u["cols"], u["e3"]
                E, SP = Ebuf[e3], SPbuf[e3]
                P.op("act", lambda e: e.activation(out=E[:, cols], in_=bank[zb][:, cols], func=AF.Exp, scale=0.125),
                     reads=[PS(zb)], writes=[("E", e3)])
                P.op("act", lambda e: e.activation(out=SP[:, cols], in_=E[:, cols], func=AF.Ln, bias=1.0),
                     reads=[("E", e3)], writes=[("SP", e3)])

            def st_mm1(u):
                pb, cols, e3, first = u["pb"], u["cols"], u["e3"], u["first"]
                SP = SPbuf[e3]
                P.op("pe", lambda e: e.matmul(bank[pb][:, cols], lhsT=triU, rhs=SP[:, cols], start=first, stop=False,
                                              skip_group_check=True),
                     reads=[("SP", e3)] + CONST, writes=[PS(pb)])

            def st_mm2(u):
                if u["last"]:
                    return
                pb, cols, e3 = u["pb"], u["cols"], u["e3"]
                SP = SPbuf[e3]
                P.op("pe", lambda e: e.matmul(bank[pb][:, cols], lhsT=comp, rhs=SP[:, cols], start=False, stop=False,
                                              skip_group_check=True),
                     reads=[("SP", e3)] + CONST, writes=[PS(pb)])

            def st_ER(u):
                pb, cols, e2 = u["pb"], u["cols"], u["e2"]
                ER = ERbuf[e2]
                P.op("act", lambda e: e.activation(out=ER[:, cols], in_=bank[pb][:, cols], func=AF.Exp, scale=-1.0),
                     reads=[PS(pb)], writes=[("ER", e2)])

            def st_AT(u):
                cols, e3, e2 = u["cols"], u["e3"], u["e2"]
                E, ER, AT = Ebuf[e3], ERbuf[e2], ATbuf[e3]
                P.op("dve", lambda e: e.tensor_tensor(out=AT[:, cols], in0=E[:, cols], in1=ER[:, cols], op=ALU.mult),
                     reads=[("E", e3), ("ER", e2)], writes=[("AT", e3)])

            def st_AV(u):
                cols, e3, sb, h, pr, ob, first, last = u["cols"], u["e3"], u["sb"], u["h"], u["pr"], u["ob"], u["first"], u["last"]
                AT = ATbuf[e3]
                hp0 = (h // 2) * 128
                P.op("pe", lambda e: e.matmul(bank[ob][:, cols], lhsT=vs[:, sb, hp0:hp0 + 128], rhs=AT[:, cols],
                                              start=first, stop=last, skip_group_check=True),
                     reads=[("AT", e3), ("vs", sb)], writes=[PS(ob)])
                for _d in range(NDUMMY):
                    P.op("pe", lambda e: e.matmul(bank[7], lhsT=triU, rhs=ATbuf[e3][:, 0:512], start=True, stop=True,
                                                  skip_group_check=True),
                         reads=CONST, writes=[PS(7)])
                if last:
                    qc, t0, qt = u["qc"], u["qt"] * 512, u["qt"]
                    P.op("dve", lambda e: e.tensor_copy(out=mixT[pr:pr + 64, 4 + qc, t0:t0 + 512], in_=bank[ob][pr:pr + 64, :]),
                         reads=[PS(ob)], writes=[("mixT", "s", h, qt)])

            st_Z(units[0])
            st_Z(units[1])
            st_E_SP(units[0])
            for k in range(NU):
                if k >= 1:
                    st_mm2(units[k - 1])
                st_mm1(units[k])
                if k + 2 < NU:
                    st_Z(units[k + 2])
                if k >= 1:
                    st_AV(units[k - 1])
                if k + 1 < NU:
                    st_E_SP(units[k + 1])
                st_ER(units[k])
                st_AT(units[k])
                if sq + 1 < nseq and k % 20 == 10:
                    tt = k // 20
                    norm_transpose(x_d[sq + 1, tt * 128:(tt + 1) * 128, :], [], tt % 2, g1T, ("g1T",), hT, ("hT", tt), tt * 128)
            st_AV(units[NU - 1])

            if dbg_d is not None:
                P.op("sp", (lambda sq: lambda e: e.dma_start(out=dbg_d[sq], in_=mixT.rearrange("p a b -> p (a b)")))(sq),
                     reads=[("mixT", "m", c) for c in range(NT)] + [("mixT", "s", h, qt) for h in range(8) for qt in range(4)],
                     dma=True)

            for tt in range(NT):
                b = tt % 2
                yb = 2 * b
                xb4 = tt % 4
                P.op("sp", (lambda sq, tt, xb4: lambda e: e.dma_start(out=xt[xb4], in_=x_d[sq, tt * 128:(tt + 1) * 128, :]))(sq, tt, xb4),
                     writes=[("xt", xb4)], dma=True)
                mt = [("mixT", "m", tt)] + [("mixT", "s", h, tt // 4) for h in range(8)]
                for half in range(2):
                    for kc in range(8):
                        P.op("pe", (lambda kc, tt, half, yb: lambda e: e.matmul(
                            bank[yb + half], lhsT=mixT[:, kc, tt * 128:(tt + 1) * 128],
                            rhs=wA[:, kc, half * 512:(half + 1) * 512], start=(kc == 0), stop=(kc == 7)))(kc, tt, half, yb),
                            reads=[("wA", kc)] + mt, writes=[PS(yb + half)])
                me = post_norm_residual(yb, b, g2b, ("g2b",), [], out_d[sq, tt * 128:(tt + 1) * 128, :], [("out", sq, tt)],
                                        xb=xb4)
                if not do_ffn:
                    fin.append(me)

        if do_ffn:
            R1NAMES = ["hT", "qk", "raw0", "raw1", "diagM", "vs", "rows", "wB", "E", "ER", "SP", "AT"]
            R2NAMES = ["mixT", "vst", "wA"]
            R3NAMES = ["g1T", "g2b", "gmb", "cwm", "cbm", "bif", "ifr", "tokS", "Cf", "Cb", "K2", "spT", "hmt", "vmt", "ogt",
                       "decb", "sm", "rsm"]
            A.off = region1_at
            wup, _ = A.take([128, 8, 2 * DFF], BF16)
            h2T, _ = A.take([128, 8, FB], BF16)
            assert A.off <= region1_end, (A.off, region1_end)
            A.off = region1_end
            wdn, _ = A.take([128, NFF, D], BF16)
            g3T, _ = A.take([128, 1024], F32)
            assert A.off <= region2_end, (A.off, region2_end)
            A.off = region2_end
            g4b, _ = A.take([128, 1024], F32)
            cwf, _ = A.take([128, NFF * 3], F32)
            cbf, _ = A.take([128, NFF], F32)
            actT, _ = A.take([128, NFF, FB], BF16)
            graw = [A.take([128, 2 + FB], F32)[0] for _ in range(2)]
            cv = [A.take([128, FB], F32)[0] for _ in range(2)]
            gel = [A.take([128, FB], BF16)[0] for _ in range(2)]
            halo, _ = A.take([128, NFF, 2], F32)
            P.alias(["wupk0", "wupk1"], ["hT"])
            P.alias(["wupk%d" % k for k in range(2, 8)] + ["h2T"], R1NAMES)
            P.alias(["wdn", "g3T"], R2NAMES)
            P.alias(["g4b", "cwf", "cbf", "actT", "graw", "cv", "gel", "halo"], R3NAMES)
            for (ap, d_ap, nm) in [(g3T, g3T_d, "g3T"), (g4b, g4b_d, "g4b"), (cwf, cwf_d, "cwf"), (cbf, cbf_d, "cbf")]:
                P.op("sp", (lambda ap, d_ap: lambda e: e.dma_start(out=ap, in_=d_ap))(ap, d_ap), writes=[(nm,)], dma=True)
            HC = (NFF // 2) * 128
            assert 2 * (2 * DFF) * 2 <= 8 * S * 2
            for kcs in ((0, 1), (2, 3, 4, 5, 6, 7)):
                for sp_ in range(2):
                    for (nm, base) in (("g", 0), ("u", DFF)):
                        c0 = base + sp_ * HC
                        for kc in kcs:
                            P.op("pool", (lambda c0, kc: lambda e: e.dma_start(
                                out=wup[:, kc, c0:c0 + HC], in_=w_up_d[kc * 128:(kc + 1) * 128, c0:c0 + HC]))(c0, kc),
                                writes=[("wupk%d" % kc, nm, sp_)], dma=True)
            for j in range(NFF):
                P.op("pool", (lambda j: lambda e: e.dma_start(out=wdn[:, j, :], in_=w_dn_d[j * 128:(j + 1) * 128, :]))(j),
                     writes=[("wdn", j)], dma=True)
            nblk_seq = S // FB
            blocks = [(sq, blk) for sq in range(nseq) for blk in range(nblk_seq)]
            NB = len(blocks)

            TPB = FB // 128

            def f_norm_a(i, tl):
                sq, blk = blocks[i]
                tt = blk * TPB + tl
                xb = tl % 2
                P.op("sp", lambda e: e.dma_start(out=xt[xb], in_=out_d[sq, tt * 128:(tt + 1) * 128, :]),
                     reads=[("out", sq, tt)], writes=[("xt", xb)], dma=True)
                rstd, rtok = rms_rstd(xt[xb], [("xt", xb)], D, 4 * xb, xh[xb], ("xh", xb))
                P.op("dve", lambda e: e.tensor_scalar(out=xh[xb], in0=xt[xb], scalar1=rstd, scalar2=None, op0=ALU.mult),
                     reads=[("xt", xb), rtok], writes=[("xh", xb)])

            def f_norm_b(i, tl):
                xb = tl % 2
                for c in range(8):
                    P.op("pe", (lambda c: lambda e: e.transpose(out=bankb[0][:, c * 128:(c + 1) * 128],
                                                                in_=xh[xb][:, c * 128:(c + 1) * 128], identity=ident_b))(c),
                         reads=[("xh", xb)] + CONST, writes=[PS(0)])
                P.op("dve", lambda e: e.tensor_tensor(
                    out=h2T[:, :, tl * 128:(tl + 1) * 128], in0=bankb[0].rearrange("p (c t) -> p c t", c=8),
                    in1=g3T.rearrange("p (c t) -> p c t", c=8), op=ALU.mult),
                    reads=[PS(0), ("g3T",)], writes=[("h2T", tl)])

            def f_up(i, mid_hook):
                sq, blk = blocks[i]
                H2 = [("h2T", tl) for tl in range(TPB)]
                if blk == 0:
                    P.op("pool", lambda e: e.memset(halo, 0.0), writes=[("halo", j) for j in range(NFF)])
                for j in range(NFF):
                    gb = j % 2
                    for kc in range(8):
                        P.op("pe", (lambda kc, j, gb: lambda e: e.matmul(
                            bank[gb][:, 0:FB], lhsT=wup[:, kc, j * 128:(j + 1) * 128], rhs=h2T[:, kc, :],
                            start=(kc == 0), stop=(kc == 7)))(kc, j, gb),
                            reads=[("wupk%d" % kc, "g", j // (NFF // 2))] + H2, writes=[PS(gb)])
                    for kc in range(8):
                        P.op("pe", (lambda kc, j, gb: lambda e: e.matmul(
                            bank[2 + gb][:, 0:FB], lhsT=wup[:, kc, DFF + j * 128:DFF + (j + 1) * 128], rhs=h2T[:, kc, :],
                            start=(kc == 0), stop=(kc == 7)))(kc, j, gb),
                            reads=[("wupk%d" % kc, "u", j // (NFF // 2))] + H2, writes=[PS(2 + gb)])
                    P.op("pool", (lambda j, gb: lambda e: e.tensor_copy(out=graw[gb][:, 0:2], in_=halo[:, j, :]))(j, gb),
                         reads=[("halo", j)], writes=[("graw", gb, "h")])
                    P.op("act", (lambda gb: lambda e: e.copy(out=graw[gb][:, 2:2 + FB], in_=bank[gb][:, 0:FB]))(gb),
                         reads=[PS(gb)], writes=[("graw", gb)])
                    P.op("pool", (lambda j, gb: lambda e: e.tensor_copy(out=halo[:, j, :], in_=graw[gb][:, FB:FB + 2]))(j, gb),
                         reads=[("graw", gb)], writes=[("halo", j)])
                    GR = [("graw", gb), ("graw", gb, "h")]
                    P.op("dve", (lambda j, gb: lambda e: e.tensor_scalar(out=cv[gb], in0=graw[gb][:, 2:2 + FB],
                                                                         scalar1=cwf[:, j * 3 + 2:j * 3 + 3], scalar2=None,
                                                                         op0=ALU.mult))(j, gb),
                         reads=GR + [("cwf",)], writes=[("cv", gb)])
                    for tp in (1, 0):
                        P.op("dve", (lambda j, gb, tp: lambda e: e.scalar_tensor_tensor(
                            out=cv[gb], in0=graw[gb][:, tp:tp + FB], scalar=cwf[:, j * 3 + tp:j * 3 + tp + 1], in1=cv[gb],
                            op0=ALU.mult, op1=ALU.add))(j, gb, tp),
                            reads=GR + [("cwf",), ("cv", gb)], writes=[("cv", gb)])
                    P.op("act", (lambda j, gb: lambda e: e.activation(out=gel[gb], in_=cv[gb], func=AF.Gelu_apprx_tanh,
                                                                      bias=cbf[:, j:j + 1]))(j, gb),
                         reads=[("cv", gb), ("cbf",)], writes=[("gel", gb)])
                    P.op("dve", (lambda j, gb: lambda e: e.tensor_tensor(out=actT[:, j, :], in0=bank[2 + gb][:, 0:FB],
                                                                         in1=gel[gb], op=ALU.mult))(j, gb),
                         reads=[PS(2 + gb), ("gel", gb)], writes=[("actT", j)])
                    if j == 9:
                        mid_hook()

            def f_down(i, tl):
                sq, blk = blocks[i]
                AT_ALL = [("actT", j) for j in range(NFF)]
                tt = blk * TPB + tl
                yb = 4 + 2 * (tl % 2)
                xb = 2 + tl % 2
                P.op("sp", lambda e: e.dma_start(out=xt[xb], in_=out_d[sq, tt * 128:(tt + 1) * 128, :]),
                     reads=[("out", sq, tt)], writes=[("xt", xb)], dma=True)
                for half in range(2):
                    for j in range(NFF):
                        P.op("pe", (lambda j, half: lambda e: e.matmul(
                            bank[yb + half], lhsT=actT[:, j, tl * 128:(tl + 1) * 128],
                            rhs=wdn[:, j, half * 512:(half + 1) * 512], start=(j == 0), stop=(j == NFF - 1)))(j, half),
                            reads=[("wdn", j)] + AT_ALL, writes=[PS(yb + half)])
                me = post_norm_residual(yb, tl % 2, g4b, ("g4b",), [], out_d[sq, tt * 128:(tt + 1) * 128, :],
                                        [("out", sq, tt)], xb=xb, junk=(cv[tl % 2].bitcast(BF16), ("cv", tl % 2)))
                fin.append(me)

            for tl in range(TPB):
                f_norm_a(0, tl)
                f_norm_b(0, tl)
            for i in range(NB):
                nxt = i + 1 < NB

                def mid(i=i, nxt=nxt):
                    if nxt:
                        f_norm_a(i + 1, 0)
                        f_norm_a(i + 1, 1)
                f_up(i, mid)
                for tl in range(TPB):
                    f_down(i, tl)
                    if nxt:
                        f_norm_b(i + 1, tl)
                        if tl + 2 < TPB:
                            f_norm_a(i + 1, tl + 2)
        print("SBUF arena high-water (KB):", A.hi / 1024.0, " ops:", {e: len(P.ops[e]) for e in P.ENGS})
        P.emit(final_waits=fin)
    return nc


_CACHE = {}


def _prep_small(inputs):
    f = lambda a: np.ascontiguousarray(np.asarray(a, dtype=np.float32))
    gT = lambda g: f(np.broadcast_to(g.reshape(8, 128).T[:, :, None], (128, 8, 128)).reshape(128, 1024))
    gb = lambda g: f(np.broadcast_to(g.reshape(1, -1), (128, g.size)))
    d = {}
    d["g1T"] = gT(np.asarray(inputs["pre_mix_norm"])[0])
    d["g3T"] = gT(np.asarray(inputs["pre_ffn_norm"])[0])
    d["g2b"] = gb(np.asarray(inputs["post_mix_norm"])[0])
    d["g4b"] = gb(np.asarray(inputs["post_ffn_norm"])[0])
    d["gmb"] = gb(np.asarray(inputs["mlstm_norm"])[0])
    cw = np.asarray(inputs["mlstm_conv_w"])[0]
    d["cwm"] = f(cw.reshape(4, 8, 128).transpose(2, 1, 0).reshape(128, 32))
    d["cbm"] = f(np.asarray(inputs["mlstm_conv_b"])[0].reshape(8, 128).T)
    d["bif"] = f(np.stack([np.asarray(inputs["mlstm_b_i"])[0], np.asarray(inputs["mlstm_b_f"])[0]], axis=1))
    fw = np.asarray(inputs["ffn_conv_w"])[0]
    d["cwf"] = f(fw.reshape(3, NFF, 128).transpose(2, 1, 0).reshape(128, NFF * 3))
    d["cbf"] = f(np.asarray(inputs["ffn_conv_b"])[0].reshape(NFF, 128).T)
    return d


def kernel(**inputs):
    x = np.asarray(inputs["x"], dtype=np.float32)
    nseq = x.shape[0] // NCORES
    if "nc" not in _CACHE:
        _CACHE["nc"] = build_program(nseq=nseq)
    nc = _CACHE["nc"]
    small = _prep_small(inputs)
    shared = {
        "w_in": np.ascontiguousarray(np.asarray(inputs["w_in"], dtype=np.float32)[0]),
        "w_out": np.ascontiguousarray(np.asarray(inputs["w_out"], dtype=np.float32)[0]),
        "w_up": np.ascontiguousarray(np.asarray(inputs["w_up"], dtype=np.float32)[0]),
        "w_down": np.ascontiguousarray(np.asarray(inputs["w_down"], dtype=np.float32)[0]),
    }
    shared.update(small)
    in_maps = []
    for c in range(NCORES):
        m = dict(shared)
        m["x"] = np.ascontiguousarray(x[c * nseq:(c + 1) * nseq])
        in_maps.append(m)
    res = run_bass_kernel_spmd(nc, in_maps, core_ids=list(range(NCORES)))
    return np.concatenate([np.asarray(r["out"]) for r in res.results], axis=0).astype(np.float32)
```
